# Optimizing a Trainium2 kernel written in Bass

```python
import jax
import jax.numpy as jnp
from jax import lax
import numpy as np

D_MODEL = 4096
BATCH = 4
SEQ = 2048
DEPTH = 2

CHUNK = 64
N_A_LAYERS = DEPTH // 2
N_B_LAYERS = DEPTH - N_A_LAYERS

GLA_HEADS = 8
GLA_DK = D_MODEL // 16
GLA_DV = D_MODEL // 8
GLA_GATE_RANK = 16
GLA_TAU = 16.0
GLA_QK = GLA_HEADS * GLA_DK
GLA_V = GLA_HEADS * GLA_DV
GLA_SPLITS = [GLA_QK, 2 * GLA_QK, 2 * GLA_QK + GLA_V, 2 * GLA_QK + 2 * GLA_V]
GLA_IN_COLS = 2 * GLA_QK + 2 * GLA_V + GLA_GATE_RANK

SB_HEADS = 32
SB_DH = D_MODEL // SB_HEADS
SB_QBLOCK = 128

N_EXPERTS = 32
TOP_K = 4
EXPERT_FF = 768
SWIGLU_LIMIT = 7.0
SWIGLU_ALPHA = 1.702
MOE_ROWS = 128

DN_ALPHA = (2.0 * DEPTH) ** 0.25
DN_BETA = (8.0 * DEPTH) ** -0.25
LN_EPS = 1e-5
RMS_EPS = 1e-6

kernel_name = 'yoco_gla_stickbreaking_moe_deepnorm'


def layer_norm(x, g, b):
    xf = x.astype(jnp.float32)
    xc = xf - jnp.mean(xf, axis=-1, keepdims=True)
    var = jnp.mean(xc * xc, axis=-1, keepdims=True)
    return (xc * lax.rsqrt(var + LN_EPS) * g + b).astype(x.dtype)


def gla_mixer(x, w_in, w_gate2, b_gate2, norm_g, w_out):
    bsz, seq, _ = x.shape
    n_chunks = seq // CHUNK
    q, k, v, r, g_low = jnp.split(x @ w_in, GLA_SPLITS, axis=-1)
    log_a = jax.nn.log_sigmoid((g_low @ w_gate2 + b_gate2).astype(jnp.float32)) / GLA_TAU

    def to_chunks(t, hd):
        t = t.astype(jnp.float32).reshape(bsz, n_chunks, CHUNK, GLA_HEADS, hd)
        return t.transpose(1, 0, 3, 2, 4)

    qc = to_chunks(q, GLA_DK) * (GLA_DK ** -0.5)
    kc = to_chunks(k, GLA_DK)
    vc = to_chunks(v, GLA_DV)
    cum_a = jnp.cumsum(to_chunks(log_a, GLA_DK), axis=3)
    end_a = cum_a[:, :, :, -1:, :]
    k_dec = kc * jnp.exp(end_a - cum_a)
    chunk_decay = jnp.exp(end_a[:, :, :, 0, :])

    def chunk_step(state, inp):
        k_c, v_c, q_c, dec_c = inp
        state = dec_c[..., None] * state + jnp.einsum('bhck,bhcv->bhkv', k_c, v_c)
        return state, jnp.einsum('bhck,bhkv->bhcv', q_c, state)

    state0 = jnp.zeros((bsz, GLA_HEADS, GLA_DK, GLA_DV), jnp.float32)
    _, o = lax.scan(chunk_step, state0, (k_dec, vc, qc, chunk_decay))
    o = o.transpose(1, 0, 3, 2, 4).reshape(bsz, seq, GLA_HEADS, GLA_DV)
    o = o * lax.rsqrt(jnp.mean(o * o, axis=-1, keepdims=True) + RMS_EPS) * norm_g
    o = o.reshape(bsz, seq, GLA_V) * jax.nn.silu(r.astype(jnp.float32))
    return o.astype(x.dtype) @ w_out


def shared_kv(x, w_kv):
    bsz, seq, _ = x.shape
    kv = (x @ w_kv).reshape(bsz, seq, 2, SB_HEADS, SB_DH)
    return kv[:, :, 0].transpose(0, 2, 1, 3), kv[:, :, 1].transpose(0, 2, 1, 3)


def stick_breaking_mixer(x, k, v, w_q, w_out):
    bsz, seq, _ = x.shape
    q = (x @ w_q).reshape(bsz, seq, SB_HEADS, SB_DH).transpose(0, 2, 1, 3)
    scale = SB_DH ** -0.5
    outs = []
    for blk in range(seq // SB_QBLOCK):
        lo, hi = blk * SB_QBLOCK, (blk + 1) * SB_QBLOCK
        kp, vp = k[:, :, :hi], v[:, :, :hi]
        z = jnp.einsum('bhqd,bhkd->bhqk', q[:, :, lo:hi], kp).astype(jnp.float32) * scale
        t_idx = lo + jnp.arange(SB_QBLOCK)[:, None]
        s_idx = jnp.arange(hi)[None, :]
        mask = s_idx < t_idx
        log_keep = jnp.where(mask, -jax.nn.softplus(z), 0.0)
        cum = jnp.cumsum(log_keep, axis=-1)
        log_w = jax.nn.log_sigmoid(z) + cum[..., -1:] - cum
        w = jnp.where(mask, jnp.exp(log_w), 0.0)
        outs.append(jnp.einsum('bhqk,bhkd->bhqd', w.astype(vp.dtype), vp))
    o = jnp.concatenate(outs, axis=2).transpose(0, 2, 1, 3).reshape(bsz, seq, SB_HEADS * SB_DH)
    return o @ w_out


def moe_ffn(x, w_r, b_r, w_gu, b_gu, w_dn, b_dn):
    bsz, seq, dm = x.shape
    n_tok = bsz * seq
    xf = x.reshape(n_tok, dm)
    logits = (xf @ w_r).astype(jnp.float32) + b_r.astype(jnp.float32)
    top_val, top_idx = lax.top_k(logits, TOP_K)
    gates = jax.nn.softmax(top_val, axis=-1)

    e_flat = top_idx.reshape(-1).astype(jnp.int32)
    tok_flat = jnp.repeat(jnp.arange(n_tok, dtype=jnp.int32), TOP_K)
    g_flat = gates.reshape(-1)
    order = jnp.argsort(e_flat)
    e_sorted, tok_sorted, g_sorted = e_flat[order], tok_flat[order], g_flat[order]

    counts = jnp.bincount(e_flat, length=N_EXPERTS).astype(jnp.int32)
    padded = ((counts + MOE_ROWS - 1) // MOE_ROWS) * MOE_ROWS
    start = jnp.cumsum(counts) - counts
    pend = jnp.cumsum(padded)
    pstart = pend - padded
    n_assign = n_tok * TOP_K
    rank = jnp.arange(n_assign, dtype=jnp.int32) - start[e_sorted]
    dest = pstart[e_sorted] + rank
    n_rows = n_assign + N_EXPERTS * MOE_ROWS
    n_blocks = n_rows // MOE_ROWS
    row_tok = jnp.full((n_rows,), n_tok, jnp.int32).at[dest].set(tok_sorted)
    row_gate = jnp.zeros((n_rows,), jnp.float32).at[dest].set(g_sorted)
    blk_expert = jnp.clip(jnp.searchsorted(pend, jnp.arange(n_blocks, dtype=jnp.int32) * MOE_ROWS,
                                           side='right'), 0, N_EXPERTS - 1)
    x_pad = jnp.concatenate([xf, jnp.zeros((1, dm), xf.dtype)], axis=0)

    def expert_block(args):
        e, toks, g = args
        h = x_pad[toks] @ w_gu[e] + b_gu[e]
        h_glu = jnp.minimum(h[:, :EXPERT_FF], SWIGLU_LIMIT)
        h_lin = jnp.clip(h[:, EXPERT_FF:], -SWIGLU_LIMIT, SWIGLU_LIMIT)
        act = h_glu * jax.nn.sigmoid(SWIGLU_ALPHA * h_glu) * (h_lin + 1.0)
        y = act @ w_dn[e] + b_dn[e]
        return y * g[:, None].astype(y.dtype)

    ys = lax.map(expert_block, (blk_expert, row_tok.reshape(n_blocks, MOE_ROWS),
                                row_gate.reshape(n_blocks, MOE_ROWS)))
    out = jnp.zeros((n_tok + 1, dm), ys.dtype).at[row_tok].add(ys.reshape(n_rows, dm))
    return out[:n_tok].reshape(bsz, seq, dm).astype(x.dtype)


def setup_inputs(seed: int = 0) -> dict:
    key = jax.random.key(seed)
    ks = jax.random.split(key, 24)
    f32 = jnp.float32
    dm = D_MODEL

    def nrm(k, shape, scale):
        return jax.random.normal(k, shape, f32) * scale

    gla_col_scale = jnp.concatenate([jnp.ones((2 * GLA_QK,), f32), jnp.full((GLA_V,), DN_BETA, f32),
                                     jnp.ones((GLA_V + GLA_GATE_RANK,), f32)])
    kv_col_scale = jnp.concatenate([jnp.ones((dm,), f32), jnp.full((dm,), DN_BETA, f32)])
    return {
        'x': nrm(ks[0], (BATCH, SEQ, dm), 1.0),
        'gla_w_in': nrm(ks[1], (N_A_LAYERS, dm, GLA_IN_COLS), dm ** -0.5) * gla_col_scale,
        'gla_w_gate2': nrm(ks[2], (N_A_LAYERS, GLA_GATE_RANK, GLA_QK), GLA_GATE_RANK ** -0.5),
        'gla_b_gate2': nrm(ks[3], (N_A_LAYERS, GLA_QK), 0.1),
        'gla_norm_g': 1.0 + nrm(ks[4], (N_A_LAYERS, GLA_DV), 0.02),
        'gla_w_out': nrm(ks[5], (N_A_LAYERS, GLA_V, dm), GLA_V ** -0.5 * DN_BETA),
        'sb_w_q': nrm(ks[6], (N_B_LAYERS, dm, SB_HEADS * SB_DH), dm ** -0.5),
        'sb_w_out': nrm(ks[7], (N_B_LAYERS, SB_HEADS * SB_DH, dm), (SB_HEADS * SB_DH) ** -0.5 * DN_BETA),
        'shared_w_kv': nrm(ks[8], (dm, 2 * dm), dm ** -0.5) * kv_col_scale,
        'router_w': nrm(ks[9], (DEPTH, dm, N_EXPERTS), dm ** -0.5),
        'router_b': nrm(ks[10], (DEPTH, N_EXPERTS), 0.01),
        'moe_w_gate_up': nrm(ks[11], (DEPTH, N_EXPERTS, dm, 2 * EXPERT_FF), dm ** -0.5),
        'moe_b_gate_up': nrm(ks[12], (DEPTH, N_EXPERTS, 2 * EXPERT_FF), 0.01),
        'moe_w_down': nrm(ks[13], (DEPTH, N_EXPERTS, EXPERT_FF, dm), EXPERT_FF ** -0.5 * DN_BETA),
        'moe_b_down': nrm(ks[14], (DEPTH, N_EXPERTS, dm), 0.01),
        'ln1_g': 1.0 + nrm(ks[15], (DEPTH, dm), 0.02),
        'ln1_b': nrm(ks[16], (DEPTH, dm), 0.02),
        'ln2_g': 1.0 + nrm(ks[17], (DEPTH, dm), 0.02),
        'ln2_b': nrm(ks[18], (DEPTH, dm), 0.02),
    }


def reference(x, gla_w_in, gla_w_gate2, gla_b_gate2, gla_norm_g, gla_w_out, sb_w_q, sb_w_out,
              shared_w_kv, router_w, router_b, moe_w_gate_up, moe_b_gate_up, moe_w_down, moe_b_down,
              ln1_g, ln1_b, ln2_g, ln2_b):
    k_sh, v_sh = None, None
    for layer in range(DEPTH):
        if layer < N_A_LAYERS:
            mix = gla_mixer(x, gla_w_in[layer], gla_w_gate2[layer], gla_b_gate2[layer],
                            gla_norm_g[layer], gla_w_out[layer])
        else:
            if layer == N_A_LAYERS:
                k_sh, v_sh = shared_kv(x, shared_w_kv)
            j = layer - N_A_LAYERS
            mix = stick_breaking_mixer(x, k_sh, v_sh, sb_w_q[j], sb_w_out[j])
        x = layer_norm(DN_ALPHA * x + mix, ln1_g[layer], ln1_b[layer])
        ffn = moe_ffn(x, router_w[layer], router_b[layer], moe_w_gate_up[layer], moe_b_gate_up[layer],
                      moe_w_down[layer], moe_b_down[layer])
        x = layer_norm(DN_ALPHA * x + ffn, ln2_g[layer], ln2_b[layer])
    return x
```

```python
import numpy as np
import ml_dtypes
from contextlib import ExitStack
import concourse.bass as bass
import concourse.mybir as mybir
from concourse.bass_utils import run_bass_kernel_spmd

F32 = mybir.dt.float32
BF16 = mybir.dt.bfloat16
ALU = mybir.AluOpType
AF = mybir.ActivationFunctionType

D = 4096
KC = 32
NT = 1024
NH_GLA = 8
DK = 256
DV = 512
QK = 2048
GV = 4096
GIN = 12304
TAU = 16.0
NE = 32
FF = 768
DEPTH = 2
ALPHA = (2.0 * DEPTH) ** 0.25
LN_EPS = 1e-5
RMS_EPS = 1e-6
SB_H = 32
SB_DH = 128


class Buf:
    __slots__ = ("name", "ap", "last_w", "reads", "dsem")

    def __init__(self, name, ap=None):
        self.name = name
        self.ap = ap
        self.last_w = None
        self.reads = {}
        self.dsem = None

    def __getitem__(self, idx):
        return self.ap[idx]


class Sched:
    ENG = ("sp", "pe", "act", "dve", "pool")

    def __init__(self, nc):
        self.nc = nc
        self.streams = {e: [] for e in self.ENG}
        self.count = {}
        self.seen = {e: {} for e in self.ENG}
        self.sem_keys = []
        self.sems = {}
        self.es = ExitStack()
        self.free_keys = []
        self.dsem_bufs = []
        for e in ("pe", "act", "dve", "pool"):
            self._new_sem("E_" + e)

    def _new_sem(self, key):
        self.sem_keys.append(key)
        self.count[key] = 0
        return key

    def _waits(self, eng, reads, writes):
        need = {}

        def add(sig):
            if sig is None:
                return
            k, v = sig
            if need.get(k, 0) < v:
                need[k] = v
        for b in reads:
            add(b.last_w)
        for b in writes:
            add(b.last_w)
            for k, v in b.reads.items():
                add((k, v))
        out = []
        seen = self.seen[eng]
        for k, v in need.items():
            if k == "E_pe" and eng == "pe":
                continue
            if seen.get(k, 0) >= v:
                continue
            seen[k] = v
            out.append((k, v))
        return out

    def _mark(self, sig, reads, writes):
        key, val = sig
        for b in reads:
            if b.reads.get(key, 0) < val:
                b.reads[key] = val
        for b in writes:
            b.last_w = sig
            b.reads = {}

    def op(self, eng, fn, reads=(), writes=()):
        waits = self._waits(eng, reads, writes)
        key = "E_" + eng
        self.count[key] += 1
        sig = (key, self.count[key])
        self.streams[eng].append((waits, fn, key, 1, 1))
        self._mark(sig, reads, writes)
        return sig

    def dma(self, fn, reads=(), writes=(), q="sp", n=1, amt=16):
        dst = writes[0]
        if dst.dsem is None:
            if self.free_keys:
                dst.dsem = self.free_keys.pop()
            else:
                dst.dsem = self._new_sem("D%d" % len(self.sem_keys))
            self.dsem_bufs.append(dst)
        key = dst.dsem
        waits = self._waits(q, reads, writes)
        self.count[key] += amt * n
        sig = (key, self.count[key])
        self.streams[q].append((waits, fn, key, amt, n))
        self._mark(sig, reads, writes)
        return sig

    def flush(self):
        nc = self.nc
        for e in self.ENG:
            waits = []
            for k in self.sem_keys:
                v = self.count[k]
                if v > 0 and self.seen[e].get(k, 0) < v:
                    self.seen[e][k] = v
                    waits.append((k, v))
            self.streams[e].append((waits, None, None, 0, 0))
        for k in self.sem_keys:
            if k not in self.sems and self.count[k] > 0:
                self.sems[k] = self.es.enter_context(nc.semaphore(k))
        sems = self.sems

        def run(stream):
            def body(eng):
                for waits, fn, key, amt, n in stream:
                    for (k, v) in waits:
                        eng.wait_ge(sems[k], v)
                    if fn is None:
                        continue
                    r = fn(eng)
                    if not isinstance(r, (list, tuple)):
                        r = [r]
                    assert len(r) == n, (len(r), n)
                    for ins in r:
                        ins.then_inc(sems[key], amt)
            return body
        with nc.Block() as block:
            block.sync(run(self.streams["sp"]))
            block.tensor(run(self.streams["pe"]))
            block.scalar(run(self.streams["act"]))
            block.vector(run(self.streams["dve"]))
            block.gpsimd(run(self.streams["pool"]))
        self.streams = {e: [] for e in self.ENG}
        for b in self.dsem_bufs:
            self.free_keys.append(b.dsem)
            b.dsem = None
        self.dsem_bufs = []

    def close(self):
        self.es.close()


class Prog:
    def __init__(self, layers):
        self.layers = layers
        self.nc = bass.Bass("TRN2", target_bir_lowering=False)
        self.S = Sched(self.nc)
        self.dram = {}
        self.out_names = []

    def din(self, name, shape, dt=F32):
        t = self.nc.dram_tensor(name, list(shape), dt, kind="ExternalInput").ap()
        self.dram[name] = Buf(name, t)
        return self.dram[name]

    def dout(self, name, shape, dt=F32):
        t = self.nc.dram_tensor(name, list(shape), dt, kind="ExternalOutput").ap()
        self.dram[name] = Buf(name, t)
        self.out_names.append(name)
        return self.dram[name]

    def dscr(self, name, shape, dt):
        t = self.nc.dram_tensor(name, list(shape), dt).ap()
        self.dram[name] = Buf(name, t)
        return self.dram[name]

    def sb(self, es, name, shape, dt):
        self.uid = getattr(self, "uid", 0) + 1
        name = "%s_u%d" % (name, self.uid)
        return Buf(name, es.enter_context(self.nc.sbuf_tensor(name, list(shape), dt)))

    def ps(self, es, name, shape, dt=F32):
        self.uid = getattr(self, "uid", 0) + 1
        name = "%s_u%d" % (name, self.uid)
        return Buf(name, es.enter_context(self.nc.psum_tensor(name, list(shape), dt)))

    def declare_consts(self):
        self.c_ident = self.din("c_ident", [128, 128])
        self.c_identb = self.din("c_identb", [128, 128], BF16)
        self.c_U = self.din("c_U", [128, 128])
        self.c_ind = self.din("c_ind", [128, 2])
        self.c_L = self.din("c_L", [128, 128])
        self.c_ones = self.din("c_ones", [128, 128])
        self.c_mask = self.din("c_mask", [128, 4, 512])
        self.c_hpbias = self.din("c_hpbias", [128, 1])

    def load(self, dst, src_buf, src_ap, q="sp"):
        self.S.dma(lambda e: e.dma_start(out=dst.ap[:], in_=src_ap), reads=[src_buf], writes=[dst], q=q)

    def linear(self, es, xT, t0, T, wbuf, w_ap, cols, mode, epilogue, wslabs, pss, sw=512):
        S = self.S
        st = self.lin_state
        for (c0, ncw) in cols:
            wb = wslabs[st["w"] % len(wslabs)]
            st["w"] += 1
            src = w_ap[:, c0:c0 + ncw].rearrange("(c p) n -> p c n", p=128)
            half = KC // 2
            S.dma(lambda e, wb=wb, src=src, ncw=ncw: [
                e.dma_start(out=wb.ap[:, 0:half, 0:ncw], in_=src[:, 0:half, :]),
                e.dma_start(out=wb.ap[:, half:KC, 0:ncw], in_=src[:, half:KC, :])],
                reads=[wbuf], writes=[wb], q="pool", n=2)
            if mode == "tm":
                for tt in range(T // 128):
                    ps = pss[st["p"] % len(pss)]
                    st["p"] += 1
                    for kc in range(KC):
                        S.op("pe", lambda e, ps=ps, wb=wb, kc=kc, tt=tt, ncw=ncw: e.matmul(
                            ps.ap[:, 0:ncw], xT.ap[:, kc, t0 + tt * 128:t0 + (tt + 1) * 128], wb.ap[:, kc, 0:ncw],
                            start=(kc == 0), stop=(kc == KC - 1)), reads=[xT, wb], writes=[ps])
                    epilogue(ps, tt, c0, ncw)
            else:
                ntg = T // 512
                for fc in range((ncw + 127) // 128):
                    nf = min(128, ncw - fc * 128)
                    pl = []
                    for tg in range(ntg):
                        pl.append(pss[st["p"] % len(pss)])
                        st["p"] += 1
                    for kc in range(KC):
                        for tg in range(ntg):
                            ps = pl[tg]
                            S.op("pe", lambda e, ps=ps, wb=wb, kc=kc, tg=tg, fc=fc, nf=nf: e.matmul(
                                ps.ap[0:nf, 0:512], wb.ap[:, kc, fc * 128:fc * 128 + nf],
                                xT.ap[:, kc, t0 + tg * 512:t0 + (tg + 1) * 512],
                                start=(kc == 0), stop=(kc == KC - 1)), reads=[xT, wb], writes=[ps])
                    for tg in range(ntg):
                        epilogue(pl[tg], c0 + fc * 128, nf, tg)

    def gla_inproj(self, xT_all, w_in):
        S = self.S
        nc = self.nc
        self.qT = self.dscr("qT", [QK, NT], BF16)
        self.k_tm = self.dscr("k_tm", [2 * NT, QK], F32)
        self.v_tm = self.dscr("v_tm", [2 * NT, GV], BF16)
        self.r_tm = self.dscr("r_tm", [NT, GV], F32)
        self.gT = self.dscr("gT", [16, 2 * NT], F32)
        w_ap = w_in.ap
        with ExitStack() as es:
            xT = self.sb(es, "xT", [128, KC, NT], BF16)
            wsl = [self.sb(es, "wsl%d" % i, [128, KC, 512], BF16) for i in range(2)]
            pss = [self.ps(es, "pp%d" % i, [128, 512]) for i in range(4)]
            stf = [self.sb(es, "stf%d" % i, [128, 512], F32) for i in range(3)]
            stb = [self.sb(es, "stb%d" % i, [128, 512], BF16) for i in range(3)]
            self.lin_state = {"w": 0, "p": 0}
            cnt = {"f": 0, "b": 0}

            for (tok0, own) in ((NT, True), (0, False)):
                src = xT_all.ap[:, tok0:tok0 + NT].rearrange("(c p) t -> p c t", p=128)
                S.dma(lambda e, src=src: [e.dma_start(out=xT.ap[:, i * 8:(i + 1) * 8, :], in_=src[:, i * 8:(i + 1) * 8, :])
                                          for i in range(4)], reads=[xT_all], writes=[xT], q="pool", n=4)

                def ep_q(ps, f0, nf, tg):
                    sbuf = stb[cnt["b"] % 3]; cnt["b"] += 1
                    S.op("act", lambda e: e.activation(sbuf.ap[:], ps.ap[:], AF.Copy, scale=DK ** -0.5), reads=[ps], writes=[sbuf])
                    S.dma(lambda e: e.dma_start(out=self.qT.ap[f0:f0 + 128, tg * 512:(tg + 1) * 512], in_=sbuf.ap[:]),
                          reads=[sbuf], writes=[self.qT])

                def ep_k(ps, tt, c0, ncw, tok0=tok0):
                    sbuf = stf[cnt["f"] % 3]; cnt["f"] += 1
                    S.op("dve", lambda e: e.tensor_copy(sbuf.ap[:], ps.ap[:]), reads=[ps], writes=[sbuf])
                    r0 = tok0 + tt * 128
                    S.dma(lambda e: e.dma_start(out=self.k_tm.ap[r0:r0 + 128, c0 - QK:c0 - QK + 512], in_=sbuf.ap[:]),
                          reads=[sbuf], writes=[self.k_tm])

                def ep_v(ps, tt, c0, ncw, tok0=tok0):
                    sbuf = stb[cnt["b"] % 3]; cnt["b"] += 1
                    S.op("act", lambda e: e.copy(sbuf.ap[:], ps.ap[:]), reads=[ps], writes=[sbuf])
                    r0 = tok0 + tt * 128
                    S.dma(lambda e: e.dma_start(out=self.v_tm.ap[r0:r0 + 128, c0 - 2 * QK:c0 - 2 * QK + 512], in_=sbuf.ap[:]),
                          reads=[sbuf], writes=[self.v_tm])

                def ep_r(ps, tt, c0, ncw):
                    sbuf = stf[cnt["f"] % 3]; cnt["f"] += 1
                    S.op("act", lambda e: e.activation(sbuf.ap[:], ps.ap[:], AF.Silu), reads=[ps], writes=[sbuf])
                    r0 = tt * 128
                    cc = c0 - 2 * QK - GV
                    S.dma(lambda e: e.dma_start(out=self.r_tm.ap[r0:r0 + 128, cc:cc + 512], in_=sbuf.ap[:]),
                          reads=[sbuf], writes=[self.r_tm])

                def ep_g(ps, f0, nf, tg, tok0=tok0):
                    sbuf = stf[cnt["f"] % 3]; cnt["f"] += 1
                    S.op("dve", lambda e: e.tensor_copy(sbuf.ap[0:16, :], ps.ap[0:16, :]), reads=[ps], writes=[sbuf])
                    S.dma(lambda e: e.dma_start(out=self.gT.ap[:, tok0 + tg * 512:tok0 + (tg + 1) * 512], in_=sbuf.ap[0:16, :]),
                          reads=[sbuf], writes=[self.gT])

                if own:
                    self.linear(es, xT, 0, NT, w_in, w_ap, [(c, 512) for c in range(0, QK, 512)], "fm", ep_q, wsl, pss)
                self.linear(es, xT, 0, NT, w_in, w_ap, [(c, 512) for c in range(QK, 2 * QK, 512)], "tm", ep_k, wsl, pss)
                self.linear(es, xT, 0, NT, w_in, w_ap, [(c, 512) for c in range(2 * QK, 2 * QK + GV, 512)], "tm", ep_v, wsl, pss)
                if own:
                    self.linear(es, xT, 0, NT, w_in, w_ap, [(c, 512) for c in range(2 * QK + GV, 2 * QK + 2 * GV, 512)], "tm", ep_r, wsl, pss)
                self.linear(es, xT, 0, NT, w_in, w_ap, [(2 * QK + 2 * GV, 16)], "fm", ep_g, wsl, pss)
            S.flush()

    def gla_scan(self, w_gate2, b_gate2, norm_g):
        S = self.S
        self.ogT = self.dscr("ogT", [GV, NT], BF16)
        with ExitStack() as es:
            sb = lambda n, s, d: self.sb(es, n, s, d)
            w2 = sb("w2", [16, QK], F32)
            b2bc = sb("b2bc", [128, QK], F32)
            gbc = sb("gbc", [128, DV], F32)
            U = sb("U", [128, 128], F32)
            ind = sb("ind", [128, 2], F32)
            identb = sb("identb", [128, 128], BF16)
            self.load(w2, w_gate2, w_gate2.ap)
            self.load(b2bc, b_gate2, b_gate2.ap.partition_broadcast(128))
            self.load(gbc, norm_g, norm_g.ap.partition_broadcast(128))
            self.load(U, self.c_U, self.c_U.ap)
            self.load(ind, self.c_ind, self.c_ind.ap)
            self.load(identb, self.c_identb, self.c_identb.ap)
            Sst = [sb("Sst%d" % i, [128, DV], F32) for i in range(16)]
            Sbf = [[sb("Sbf%d_%d" % (c, i), [128, DV], BF16) for i in range(16)] for c in range(2)]
            for i in range(16):
                S.op("pool", lambda e, i=i: e.memset(Sst[i].ap[:], 0.0), writes=[Sst[i]])
            NB = 2
            gt = [sb("gt%d" % i, [16, 128], F32) for i in range(NB)]
            kt = [sb("kt%d" % i, [128, QK], F32) for i in range(NB)]
            vt = [sb("vt%d" % i, [128, GV], BF16) for i in range(NB)]
            rt = [sb("rt0", [128, GV], F32)] * NB
            Q0 = [sb("Q0_%d" % i, [128, 16, 128], BF16) for i in range(NB)]
            Q1 = [sb("Q1_%d" % i, [128, 16, 128], BF16) for i in range(NB)]
            for i in range(NB):
                S.op("pool", lambda e, i=i: e.memset(Q0[i].ap[:], 0.0), writes=[Q0[i]])
                S.op("pool", lambda e, i=i: e.memset(Q1[i].ap[:], 0.0), writes=[Q1[i]])
            sp = sb("sp", [128, QK], F32)
            kdec = sb("kdec", [128, QK], BF16)
            t1 = [sb("t1_%d" % i, [128, 512], F32) for i in range(2)]
            t2 = [sb("t2_%d" % i, [128, 512], F32) for i in range(2)]
            dec = sb("dec", [128, 32], F32)
            og = sb("og", [128, GV], BF16)
            ogTs = sb("ogTs", [128, KC, 128], BF16)
            junk = sb("junk", [128, 512], F32)
            t3 = [sb("t3_%d" % i, [128, 512], F32) for i in range(2)]
            ssq = [sb("ssq%d" % i, [128, 1], F32) for i in range(2)]
            rstd = [sb("rstd%d" % i, [128, 1], F32) for i in range(2)]
            pA = [self.ps(es, "pA%d" % i, [128, 512]) for i in range(2)]
            pD = self.ps(es, "pD", [128, 32])
            pS = [self.ps(es, "pS%d" % i, [128, 512]) for i in range(2)]
            pO = [self.ps(es, "pO%d" % i, [128, 512]) for i in range(2)]
            pT = self.ps(es, "pT", [128, 512], BF16)
            nS = 0
            nO = 0
            for ti in range(16):
                own = ti >= 8
                bi = ti % NB
                r0 = ti * 128
                self.load(gt[bi], self.gT, self.gT.ap[:, r0:r0 + 128])
                self.load(kt[bi], self.k_tm, self.k_tm.ap[r0:r0 + 128, :])
                self.load(vt[bi], self.v_tm, self.v_tm.ap[r0:r0 + 128, :])
                if own:
                    o0 = r0 - NT
                    self.load(rt[bi], self.r_tm, self.r_tm.ap[o0:o0 + 128, :])
                    qsrc = self.qT.ap.rearrange("(c p) t -> p c t", p=128)
                    S.dma(lambda e, bi=bi, o0=o0, qsrc=qsrc: e.dma_start(out=Q0[bi].ap[:, :, 0:64], in_=qsrc[:, :, o0:o0 + 64]),
                          reads=[self.qT], writes=[Q0[bi]])
                    S.dma(lambda e, bi=bi, o0=o0, qsrc=qsrc: e.dma_start(out=Q1[bi].ap[:, :, 64:128], in_=qsrc[:, :, o0 + 64:o0 + 128]),
                          reads=[self.qT], writes=[Q1[bi]])
                for blk in range(4):
                    cs = slice(blk * 512, (blk + 1) * 512)
                    pz = pA[0]
                    pr = pA[1]
                    a1 = t1[blk % 2]
                    a2 = t2[blk % 2]
                    S.op("pe", lambda e, pz=pz, bi=bi, cs=cs: e.matmul(pz.ap[:], gt[bi].ap[:], w2.ap[:, cs], start=True, stop=True),
                         reads=[gt[bi], w2], writes=[pz])
                    S.op("dve", lambda e, pz=pz, a1=a1, cs=cs: e.tensor_tensor(a1.ap[:], pz.ap[:], b2bc.ap[:, cs], ALU.add),
                         reads=[pz, b2bc], writes=[a1])
                    S.op("act", lambda e, a1=a1: e.activation(a1.ap[:], a1.ap[:], AF.Exp, scale=-1.0), reads=[a1], writes=[a1])
                    S.op("act", lambda e, a1=a1, cs=cs: e.activation(sp.ap[:, cs], a1.ap[:], AF.Ln, bias=1.0), reads=[a1], writes=[sp])
                    S.op("pe", lambda e, pr=pr, cs=cs: e.matmul(pr.ap[:], U.ap[:], sp.ap[:, cs], start=True, stop=True),
                         reads=[U, sp], writes=[pr])
                    S.op("act", lambda e, pr=pr, a2=a2: e.activation(a2.ap[:], pr.ap[:], AF.Exp, scale=-1.0 / TAU), reads=[pr], writes=[a2])
                    S.op("dve", lambda e, a2=a2, bi=bi, cs=cs: e.tensor_tensor(kdec.ap[:, cs], kt[bi].ap[:, cs], a2.ap[:], ALU.mult),
                         reads=[kt[bi], a2], writes=[kdec])
                    for j in range(4):
                        hk = blk * 4 + j
                        S.op("pe", lambda e, hk=hk: e.matmul(pD.ap[:, 2 * hk:2 * hk + 2], sp.ap[:, hk * 128:(hk + 1) * 128], ind.ap[:],
                                                             start=True, stop=True), reads=[sp, ind], writes=[pD])
                S.op("act", lambda e: e.activation(dec.ap[:], pD.ap[:], AF.Exp, scale=-1.0 / TAU), reads=[pD], writes=[dec])
                for c in range(2):
                    rs = slice(c * 64, (c + 1) * 64)
                    for hk in range(16):
                        h = hk // 2
                        pst = pS[nS % 2]; nS += 1
                        S.op("pe", lambda e, pst=pst, rs=rs, hk=hk, h=h, bi=bi: e.matmul(
                            pst.ap[:], kdec.ap[rs, hk * 128:(hk + 1) * 128], vt[bi].ap[rs, h * DV:(h + 1) * DV], start=True, stop=True),
                            reads=[kdec, vt[bi]], writes=[pst])
                        S.op("dve", lambda e, pst=pst, hk=hk, c=c: e.scalar_tensor_tensor(
                            Sst[hk].ap[:], Sst[hk].ap[:], dec.ap[:, 2 * hk + c:2 * hk + c + 1], pst.ap[:], ALU.mult, ALU.add),
                            reads=[Sst[hk], dec, pst], writes=[Sst[hk]])
                        if own:
                            S.op("act", lambda e, hk=hk, c=c: e.copy(Sbf[c][hk].ap[:], Sst[hk].ap[:]), reads=[Sst[hk]], writes=[Sbf[c][hk]])
                if not own:
                    continue
                for h in range(NH_GLA):
                    po = pO[nO % 2]
                    ui = nO % 2
                    nO += 1
                    k = 0
                    for c in range(2):
                        Q = (Q0 if c == 0 else Q1)[bi]
                        for kh in range(2):
                            hk = 2 * h + kh
                            S.op("pe", lambda e, po=po, Q=Q, hk=hk, c=c, k=k: e.matmul(
                                po.ap[:], Q.ap[:, hk, :], Sbf[c][hk].ap[:], start=(k == 0), stop=(k == 3)),
                                reads=[Q, Sbf[c][hk]], writes=[po])
                            k += 1
                    S.op("act", lambda e, po=po, ui=ui: e.activation(junk.ap[:], po.ap[:], AF.Square, accum_out=ssq[ui].ap[:]),
                         reads=[po], writes=[junk, ssq[ui]])
                    S.op("dve", lambda e, ui=ui: e.tensor_scalar(rstd[ui].ap[:], ssq[ui].ap[:], 1.0 / DV, RMS_EPS, ALU.mult, ALU.add),
                         reads=[ssq[ui]], writes=[rstd[ui]])
                    S.op("act", lambda e, ui=ui: e.activation(rstd[ui].ap[:], rstd[ui].ap[:], AF.Sqrt), reads=[rstd[ui]], writes=[rstd[ui]])
                    S.op("dve", lambda e, ui=ui: e.reciprocal(rstd[ui].ap[:], rstd[ui].ap[:]), reads=[rstd[ui]], writes=[rstd[ui]])
                    S.op("dve", lambda e, po=po, ui=ui: e.scalar_tensor_tensor(
                        t3[ui].ap[:], po.ap[:], rstd[ui].ap[:], gbc.ap[:], ALU.mult, ALU.mult), reads=[po, rstd[ui], gbc], writes=[t3[ui]])
                    S.op("pool", lambda e, ui=ui, h=h, bi=bi: e.tensor_tensor(
                        og.ap[:, h * DV:(h + 1) * DV], t3[ui].ap[:], rt[bi].ap[:, h * DV:(h + 1) * DV], ALU.mult),
                        reads=[t3[ui], rt[bi]], writes=[og])
                for g4 in range(8):
                    for j in range(4):
                        fc = g4 * 4 + j
                        S.op("pe", lambda e, fc=fc, j=j: e.transpose(pT.ap[:, j * 128:(j + 1) * 128], og.ap[:, fc * 128:(fc + 1) * 128], identb.ap[:]),
                             reads=[og, identb], writes=[pT])
                    S.op("act", lambda e, g4=g4: e.copy(ogTs.ap[:, g4 * 4:(g4 + 1) * 4, :], pT.ap[:].rearrange("p (a t) -> p a t", a=4)),
                         reads=[pT], writes=[ogTs])
                o0 = r0 - NT
                S.dma(lambda e, o0=o0: e.dma_start(out=self.ogT.ap.rearrange("(c p) t -> p c t", p=128)[:, :, o0:o0 + 128], in_=ogTs.ap[:]),
                      reads=[ogTs], writes=[self.ogT])
            S.flush()

    def outproj_residual(self, aT_dram, w, x_tm, tag):
        S = self.S
        y = self.dscr("y_" + tag, [NT, D], F32)
        with ExitStack() as es:
            xT = self.sb(es, "xT", [128, KC, NT], BF16)
            wsl = [self.sb(es, "wsl%d" % i, [128, KC, 512], BF16) for i in range(2)]
            pss = [self.ps(es, "pp%d" % i, [128, 512]) for i in range(4)]
            xs = [self.sb(es, "xs%d" % i, [128, 512], F32) for i in range(3)]
            self.lin_state = {"w": 0, "p": 0}
            cnt = {"x": 0}
            src = aT_dram.ap.rearrange("(c p) t -> p c t", p=128)
            S.dma(lambda e: [e.dma_start(out=xT.ap[:, i * 8:(i + 1) * 8, :], in_=src[:, i * 8:(i + 1) * 8, :]) for i in range(4)],
                  reads=[aT_dram], writes=[xT], n=4)

            def ep(ps, tt, c0, ncw):
                xb = xs[cnt["x"] % 3]; cnt["x"] += 1
                S.dma(lambda e: e.dma_start(out=xb.ap[:], in_=x_tm.ap[tt * 128:(tt + 1) * 128, c0:c0 + 512]), reads=[x_tm], writes=[xb])
                S.op("dve", lambda e: e.scalar_tensor_tensor(xb.ap[:], xb.ap[:], ALPHA, ps.ap[:], ALU.mult, ALU.add), reads=[xb, ps], writes=[xb])
                S.dma(lambda e: e.dma_start(out=y.ap[tt * 128:(tt + 1) * 128, c0:c0 + 512], in_=xb.ap[:]), reads=[xb], writes=[y])
            self.linear(es, xT, 0, NT, w, w.ap, [(c, 512) for c in range(0, D, 512)], "tm", ep, wsl, pss)
            S.flush()
        return y

    def ln_phase(self, y, g, b, x_out, xT_out=None, router=None, tag="", xT_pm=None):
        S = self.S
        if router is not None:
            w_r, b_r, b_dn = router
            self.gates = self.dscr("gates_" + tag, [NT, NE], F32)
            self.accinit = self.dscr("accinit_" + tag, [NT, D], F32)
        with ExitStack() as es:
            sb = lambda n, s, d: self.sb(es, n, s, d)
            gbc = sb("lng", [128, D], F32)
            bbc = sb("lnb", [128, D], F32)
            identb = sb("identb", [128, 128], BF16)
            self.load(gbc, g, g.ap.partition_broadcast(128))
            self.load(bbc, b, b.ap.partition_broadcast(128))
            self.load(identb, self.c_identb, self.c_identb.ap)
            yt = [sb("yt%d" % i, [128, D], F32) for i in range(2)]
            xo = [sb("xo%d" % i, [128, D], F32) for i in range(2)]
            if router is not None:
                wr = sb("wr", [128, KC, NE], F32)
                whi = sb("whi", [128, KC, NE], BF16)
                wlo = sb("wlo", [128, KC, NE], BF16)
                brbc = sb("brbc", [128, NE], F32)
                self.load(wr, w_r, w_r.ap.rearrange("(c p) e -> p c e", p=128))
                self.load(brbc, b_r, b_r.ap.partition_broadcast(128))
                S.op("dve", lambda e: e.tensor_copy(whi.ap[:], wr.ap[:]), reads=[wr], writes=[whi])
                S.op("dve", lambda e: e.tensor_tensor(wlo.ap[:], wr.ap[:], whi.ap[:], ALU.subtract), reads=[wr, whi], writes=[wlo])
                bdh = sb("bdh", [NE, D], BF16)
                bdl = sb("bdl", [NE, D], BF16)
                bdf = yt[1]
                S.dma(lambda e: e.dma_start(out=bdf.ap[0:NE, :], in_=b_dn.ap), reads=[b_dn], writes=[bdf])
                S.op("dve", lambda e: e.tensor_copy(bdh.ap[:], bdf.ap[0:NE, :]), reads=[bdf], writes=[bdh])
                S.op("dve", lambda e: e.tensor_tensor(bdl.ap[:], bdf.ap[0:NE, :], bdh.ap[:], ALU.subtract), reads=[bdf, bdh], writes=[bdl])
                pB = [self.ps(es, "pB%d" % i, [128, 512]) for i in range(2)]
                pL = self.ps(es, "pL", [128, NE])
                pG = self.ps(es, "pG", [NE, 256], BF16)
                xTl = sb("xTl", [128, KC, 128], BF16)
                olo = sb("olo", [128, D], BF16)
                lg = sb("lg", [128, NE], F32)
                ex = sb("ex", [128, NE], F32)
                mk = sb("mk", [128, NE], F32)
                top8 = sb("top8", [128, 8], F32)
                nm1 = sb("nm1", [128, 1], F32)
                den = sb("den", [128, 1], F32)
                gts = sb("gts", [128, NE], F32)
                ghl = sb("ghl", [128, 2, NE], BF16)
                gTh = sb("gTh", [NE, 256], BF16)
            stats = sb("stats", [128, 8, 6], F32)
            mv = sb("mv", [128, 2], F32)
            rs = sb("rs", [128, 1], F32)
            ohi = sb("ohi", [128, D], BF16)
            xTb = sb("xTb", [128, KC, 128], BF16)
            pT = [self.ps(es, "pT%d" % i, [128, 512], BF16) for i in range(2)]
            ncp = 0
            for tt in range(NT // 128):
                a = yt[tt % 2]
                o = xo[tt % 2]
                r0 = tt * 128
                self.load(a, y, y.ap[r0:r0 + 128, :])
                for c in range(8):
                    S.op("dve", lambda e, a=a, c=c: e.bn_stats(stats.ap[:, c, :], a.ap[:, c * 512:(c + 1) * 512]), reads=[a], writes=[stats])
                S.op("dve", lambda e: e.bn_aggr(mv.ap[:], stats.ap[:].rearrange("p a b -> p (a b)")), reads=[stats], writes=[mv])
                S.op("dve", lambda e: e.tensor_scalar(rs.ap[:], mv.ap[:, 1:2], LN_EPS, None, ALU.add), reads=[mv], writes=[rs])
                S.op("act", lambda e: e.activation(rs.ap[:], rs.ap[:], AF.Sqrt), reads=[rs], writes=[rs])
                S.op("dve", lambda e: e.reciprocal(rs.ap[:], rs.ap[:]), reads=[rs], writes=[rs])
                S.op("dve", lambda e, a=a: e.tensor_scalar(a.ap[:], a.ap[:], mv.ap[:, 0:1], rs.ap[:], ALU.subtract, ALU.mult),
                     reads=[a, mv, rs], writes=[a])
                S.op("dve", lambda e, a=a: e.tensor_tensor(a.ap[:], a.ap[:], gbc.ap[:], ALU.mult), reads=[a, gbc], writes=[a])
                S.op("pool", lambda e, a=a, o=o: e.tensor_tensor(o.ap[:], a.ap[:], bbc.ap[:], ALU.add), reads=[a, bbc], writes=[o])
                S.dma(lambda e, o=o, r0=r0: e.dma_start(out=x_out.ap[r0:r0 + 128, :], in_=o.ap[:]), reads=[o], writes=[x_out])
                if xT_out is None and xT_pm is None:
                    continue
                S.op("act", lambda e, o=o: e.copy(ohi.ap[:], o.ap[:]), reads=[o], writes=[ohi])
                srcs = [(ohi, xTb)]
                if router is not None:
                    S.op("dve", lambda e, o=o: e.tensor_tensor(olo.ap[:], o.ap[:], ohi.ap[:], ALU.subtract), reads=[o, ohi], writes=[olo])
                    srcs.append((olo, xTl))
                for (src_, dst_) in srcs:
                    for g4 in range(8):
                        p = pT[ncp % 2]
                        for j in range(4):
                            fc = g4 * 4 + j
                            S.op("pe", lambda e, p=p, src_=src_, fc=fc, j=j: e.transpose(
                                p.ap[:, j * 128:(j + 1) * 128], src_.ap[:, fc * 128:(fc + 1) * 128], identb.ap[:]), reads=[src_, identb], writes=[p])
                        eng = "act" if ncp % 2 == 0 else "dve"
                        ncp += 1
                        if eng == "act":
                            S.op("act", lambda e, p=p, g4=g4, dst_=dst_: e.copy(dst_.ap[:, g4 * 4:(g4 + 1) * 4, :], p.ap[:].rearrange("p (a t) -> p a t", a=4)),
                                 reads=[p], writes=[dst_])
                        else:
                            S.op("dve", lambda e, p=p, g4=g4, dst_=dst_: e.tensor_copy(dst_.ap[:, g4 * 4:(g4 + 1) * 4, :], p.ap[:].rearrange("p (a t) -> p a t", a=4)),
                                 reads=[p], writes=[dst_])
                if xT_pm is not None:
                    for k4 in range(4):
                        S.dma(lambda e, r0=r0, k4=k4: e.dma_start(out=xT_pm[k4].ap.rearrange("p (c t) -> p c t", c=8)[:, :, r0:r0 + 128],
                                                                 in_=xTb.ap[:, 8 * k4:8 * k4 + 8, :]), reads=[xTb], writes=[xT_pm[k4]])
                else:
                    S.dma(lambda e, r0=r0: e.dma_start(out=xT_out.ap.rearrange("(c p) t -> p c t", p=128)[:, :, r0:r0 + 128], in_=xTb.ap[:]),
                          reads=[xTb], writes=[xT_out])
                if router is None:
                    continue
                trip = [(xTb, whi), (xTl, whi), (xTb, wlo)]
                k = 0
                for (xa, wa) in trip:
                    for kc in range(KC):
                        S.op("pe", lambda e, xa=xa, wa=wa, kc=kc, k=k: e.matmul(pL.ap[:], xa.ap[:, kc, :], wa.ap[:, kc, :], start=(k == 0), stop=(k == 3 * KC - 1)),
                             reads=[xa, wa], writes=[pL])
                        k += 1
                S.op("dve", lambda e: e.tensor_tensor(lg.ap[:], pL.ap[:], brbc.ap[:], ALU.add), reads=[pL, brbc], writes=[lg])
                S.op("dve", lambda e: e.max(top8.ap[:], lg.ap[:]), reads=[lg], writes=[top8])
                S.op("dve", lambda e: e.tensor_scalar(mk.ap[:], lg.ap[:], top8.ap[:, 3:4], None, ALU.is_ge), reads=[lg, top8], writes=[mk])
                S.op("dve", lambda e: e.tensor_scalar(nm1.ap[:], top8.ap[:, 0:1], -1.0, None, ALU.mult), reads=[top8], writes=[nm1])
                S.op("act", lambda e: e.activation(ex.ap[:], lg.ap[:], AF.Exp, bias=nm1.ap[:]), reads=[lg, nm1], writes=[ex])
                S.op("dve", lambda e: e.tensor_tensor(ex.ap[:], ex.ap[:], mk.ap[:], ALU.mult), reads=[ex, mk], writes=[ex])
                S.op("dve", lambda e: e.reduce_sum(den.ap[:], ex.ap[:], mybir.AxisListType.X), reads=[ex], writes=[den])
                S.op("dve", lambda e: e.reciprocal(den.ap[:], den.ap[:]), reads=[den], writes=[den])
                S.op("dve", lambda e: e.tensor_scalar(gts.ap[:], ex.ap[:], den.ap[:], None, ALU.mult), reads=[ex, den], writes=[gts])
                S.dma(lambda e, r0=r0: e.dma_start(out=self.gates.ap[r0:r0 + 128, :], in_=gts.ap[:]), reads=[gts], writes=[self.gates])
                S.op("dve", lambda e: e.tensor_copy(ghl.ap[:, 0, :], gts.ap[:]), reads=[gts], writes=[ghl])
                S.op("dve", lambda e: e.tensor_tensor(ghl.ap[:, 1, :], gts.ap[:], ghl.ap[:, 0, :], ALU.subtract), reads=[gts, ghl], writes=[ghl])
                for q_ in range(2):
                    S.op("pe", lambda e, q_=q_: e.transpose(pG.ap[:, q_ * 128:(q_ + 1) * 128], ghl.ap[:, q_, :], identb.ap[:]), reads=[ghl, identb], writes=[pG])
                S.op("act", lambda e: e.copy(gTh.ap[:], pG.ap[:]), reads=[pG], writes=[gTh])
                for dc in range(8):
                    p = pB[dc % 2]
                    cs = slice(dc * 512, (dc + 1) * 512)
                    S.op("pe", lambda e, p=p, cs=cs: e.matmul(p.ap[:], gTh.ap[:, 0:128], bdh.ap[:, cs], start=True, stop=False), reads=[gTh, bdh], writes=[p])
                    S.op("pe", lambda e, p=p, cs=cs: e.matmul(p.ap[:], gTh.ap[:, 128:256], bdh.ap[:, cs], start=False, stop=False), reads=[gTh, bdh], writes=[p])
                    S.op("pe", lambda e, p=p, cs=cs: e.matmul(p.ap[:], gTh.ap[:, 0:128], bdl.ap[:, cs], start=False, stop=True), reads=[gTh, bdl], writes=[p])
                    S.op("act", lambda e, p=p, cs=cs, a=a: e.copy(a.ap[:, cs], p.ap[:]), reads=[p], writes=[a])
                S.dma(lambda e, r0=r0, a=a: e.dma_start(out=self.accinit.ap[r0:r0 + 128, :], in_=a.ap[:]), reads=[a], writes=[self.accinit])
            S.flush()

    def moe(self, x1T, x1, w_gu, b_guT, w_dn, tag):
        actT_all = self.moe_gateup(x1T, w_gu, b_guT, tag)
        return self.moe_down(actT_all, x1, w_dn, tag)

    def moe_gateup(self, x1T, w_gu, b_guT, tag):
        S = self.S
        actT_all = self.dscr("actT_" + tag, [NE, FF, NT], BF16)
        with ExitStack() as es:
            sb = lambda n, s, d: self.sb(es, n, s, d)
            xT = sb("mxT", [128, KC, NT], BF16)
            bgu = sb("bgu", [128, NE, 12], F32)
            wg = [sb("wg%d" % i, [128, KC, 256], BF16) for i in range(3)]
            gluS = sb("gluS", [128, 6, NT], F32)
            actS = [sb("actS%d" % i, [128, 6, NT], BF16) for i in range(2)]
            g1 = [sb("g1_%d" % i, [128, 512], F32) for i in range(2)]
            sg = [sb("sg_%d" % i, [128, 512], F32) for i in range(2)]
            l1 = [sb("l1_%d" % i, [128, 512], F32) for i in range(2)]
            pH = [self.ps(es, "pH%d" % i, [128, 512]) for i in range(8)]
            self.load(bgu, b_guT, b_guT.ap)
            S.op("dve", lambda e: e.tensor_scalar(bgu.ap[:, :, 6:12], bgu.ap[:, :, 6:12], 1.0, None, ALU.add), reads=[bgu], writes=[bgu])
            src = x1T.ap.rearrange("(c p) t -> p c t", p=128)
            S.dma(lambda e: [e.dma_start(out=xT.ap[:, i * 8:(i + 1) * 8, :], in_=src[:, i * 8:(i + 1) * 8, :]) for i in range(4)],
                  reads=[x1T], writes=[xT], n=4)
            nw = 0
            nch = 0
            nev = 0
            for ex_ in range(NE):
                as_ = actS[ex_ % 2]
                for sl in range(6):
                    wb = wg[nw % 3]; nw += 1
                    wsrc = w_gu.ap[ex_, sl]
                    S.dma(lambda e, wb=wb, wsrc=wsrc: [e.dma_start(out=wb.ap[:, 0:16, :], in_=wsrc[:, 0:16, :]),
                                                       e.dma_start(out=wb.ap[:, 16:32, :], in_=wsrc[:, 16:32, :])],
                          reads=[w_gu], writes=[wb], q="pool", n=2)
                    for f2 in range(2):
                        ch = sl * 2 + f2
                        pp = [pH[(2 * nch) % 8], pH[(2 * nch + 1) % 8]]
                        nch += 1
                        for kc in range(KC):
                            for tg in range(2):
                                S.op("pe", lambda e, p=pp[tg], wb=wb, kc=kc, f2=f2, tg=tg: e.matmul(
                                    p.ap[:], wb.ap[:, kc, f2 * 128:(f2 + 1) * 128], xT.ap[:, kc, tg * 512:(tg + 1) * 512],
                                    start=(kc == 0), stop=(kc == KC - 1)), reads=[wb, xT], writes=[pp[tg]])
                        bias = bgu.ap[:, ex_, ch:ch + 1]
                        for tg in range(2):
                            p = pp[tg]
                            ts = slice(tg * 512, (tg + 1) * 512)
                            if ch < 6:
                                a = g1[nev % 2]
                                s_ = sg[nev % 2]
                                nev += 1
                                S.op("dve", lambda e, p=p, a=a, bias=bias: e.tensor_scalar(a.ap[:], p.ap[:], bias, 7.0, ALU.add, ALU.min),
                                     reads=[p, bgu], writes=[a])
                                S.op("act", lambda e, a=a, s_=s_: e.activation(s_.ap[:], a.ap[:], AF.Sigmoid, scale=1.702), reads=[a], writes=[s_])
                                S.op("dve", lambda e, a=a, s_=s_, ch=ch, ts=ts: e.tensor_tensor(gluS.ap[:, ch, ts], a.ap[:], s_.ap[:], ALU.mult),
                                     reads=[a, s_], writes=[gluS])
                            else:
                                j = ch - 6
                                a = l1[nev % 2]
                                nev += 1
                                S.op("dve", lambda e, p=p, a=a, bias=bias: e.tensor_scalar(a.ap[:], p.ap[:], bias, -6.0, ALU.add, ALU.max),
                                     reads=[p, bgu], writes=[a])
                                S.op("dve", lambda e, a=a, j=j, ts=ts, as_=as_: e.scalar_tensor_tensor(
                                    as_.ap[:, j, ts], a.ap[:], 8.0, gluS.ap[:, j, ts], ALU.min, ALU.mult), reads=[a, gluS], writes=[as_])
                S.dma(lambda e, as_=as_, ex_=ex_: e.dma_start(out=actT_all.ap[ex_].rearrange("(j p) t -> p j t", p=128), in_=as_.ap[:]),
                      reads=[as_], writes=[actT_all])
            S.flush()
        return actT_all

    def moe_down(self, actT_all, x1, w_dn, tag):
        S = self.S
        y2 = self.dscr("y2_" + tag, [NT, D], F32)
        DH = D // 2
        with ExitStack() as es:
            sb = lambda n, s, d: self.sb(es, n, s, d)
            acc = sb("acc", [128, NT // 128, DH], F32)
            gts = sb("mg", [128, NT // 128, NE], F32)
            at = [sb("at%d" % i, [128, 6, NT], BF16) for i in range(2)]
            wd = [sb("wd%d" % i, [128, 6, DH], BF16) for i in range(2)]
            xr = [sb("xr%d" % i, [128, 512], F32) for i in range(3)]
            pY = [self.ps(es, "pY%d" % i, [128, 512]) for i in range(8)]
            self.load(gts, self.gates, self.gates.ap.rearrange("(a p) e -> p a e", p=128))
            ny = 0
            nb = 0
            nx = 0
            for dh in range(2):
                d0 = dh * DH
                self.load(acc, self.accinit, self.accinit.ap[:, d0:d0 + DH].rearrange("(a p) d -> p a d", p=128))
                for ex_ in range(NE):
                    ab = at[nb % 2]
                    wb = wd[nb % 2]
                    nb += 1
                    S.dma(lambda e, ab=ab, ex_=ex_: e.dma_start(out=ab.ap[:], in_=actT_all.ap[ex_].rearrange("(j p) t -> p j t", p=128)),
                          reads=[actT_all], writes=[ab])
                    wsrc = w_dn.ap[ex_, :, d0:d0 + DH].rearrange("(c p) n -> p c n", p=128)
                    S.dma(lambda e, wb=wb, wsrc=wsrc: [e.dma_start(out=wb.ap[:, 0:3, :], in_=wsrc[:, 0:3, :]),
                                                       e.dma_start(out=wb.ap[:, 3:6, :], in_=wsrc[:, 3:6, :])],
                          reads=[w_dn], writes=[wb], q="pool", n=2)
                    for tt in range(NT // 128):
                        for dc in range(DH // 512):
                            p = pY[ny % 8]; ny += 1
                            for j in range(6):
                                S.op("pe", lambda e, p=p, ab=ab, wb=wb, j=j, tt=tt, dc=dc: e.matmul(
                                    p.ap[:], ab.ap[:, j, tt * 128:(tt + 1) * 128], wb.ap[:, j, dc * 512:(dc + 1) * 512],
                                    start=(j == 0), stop=(j == 5)), reads=[ab, wb], writes=[p])
                            cs = slice(dc * 512, (dc + 1) * 512)
                            S.op("dve", lambda e, p=p, tt=tt, cs=cs, ex_=ex_: e.scalar_tensor_tensor(
                                acc.ap[:, tt, cs], p.ap[:], gts.ap[:, tt, ex_:ex_ + 1], acc.ap[:, tt, cs], ALU.mult, ALU.add),
                                reads=[p, gts, acc], writes=[acc])
                for tt in range(NT // 128):
                    for dc in range(DH // 512):
                        xb = xr[nx % 3]; nx += 1
                        r0 = tt * 128
                        cs = slice(dc * 512, (dc + 1) * 512)
                        gs = slice(d0 + dc * 512, d0 + (dc + 1) * 512)
                        S.dma(lambda e, xb=xb, r0=r0, gs=gs: e.dma_start(out=xb.ap[:], in_=x1.ap[r0:r0 + 128, gs]), reads=[x1], writes=[xb])
                        S.op("dve", lambda e, xb=xb, tt=tt, cs=cs: e.scalar_tensor_tensor(
                            xb.ap[:], xb.ap[:], ALPHA, acc.ap[:, tt, cs], ALU.mult, ALU.add), reads=[xb, acc], writes=[xb])
                        S.dma(lambda e, xb=xb, r0=r0, gs=gs: e.dma_start(out=y2.ap[r0:r0 + 128, gs], in_=xb.ap[:]), reads=[xb], writes=[y2])
            S.flush()
        return y2

    def moe_old(self, x1T, x1, w_gu, b_guT, w_dn, tag):
        S = self.S
        y2 = self.dscr("y2_" + tag, [NT, D], F32)
        TP = 512
        with ExitStack() as es:
            sb = lambda n, s, d: self.sb(es, n, s, d)
            xT = sb("mxT", [128, KC, TP], BF16)
            acc = sb("acc", [128, TP // 128, D], F32)
            gts = sb("mg", [128, TP // 128, NE], F32)
            bgu = sb("bgu", [128, NE, 12], F32)
            wg = [sb("wg%d" % i, [128, KC, 256], BF16) for i in range(2)]
            wd = [sb("wd%d" % i, [128, 6, 1024], BF16) for i in range(2)]
            gluS = sb("gluS", [128, 6, TP], F32)
            actT = sb("actT", [128, 6, TP], BF16)
            g1 = [sb("g1_%d" % i, [128, TP], F32) for i in range(2)]
            sg = [sb("sg_%d" % i, [128, TP], F32) for i in range(2)]
            l1 = [sb("l1_%d" % i, [128, TP], F32) for i in range(2)]
            xr = [sb("xr%d" % i, [128, 512], F32) for i in range(2)]
            pH = [self.ps(es, "pH%d" % i, [128, 512]) for i in range(4)]
            pY = [self.ps(es, "pY%d" % i, [128, 512]) for i in range(4)]
            self.load(bgu, b_guT, b_guT.ap)
            S.op("dve", lambda e: e.tensor_scalar(bgu.ap[:, :, 6:12], bgu.ap[:, :, 6:12], 1.0, None, ALU.add), reads=[bgu], writes=[bgu])
            nw = 0
            nd = 0
            nh = 0
            ny = 0
            for ps_ in range(NT // TP):
                t0 = ps_ * TP
                src = x1T.ap[:, t0:t0 + TP].rearrange("(c p) t -> p c t", p=128)
                S.dma(lambda e, src=src: [e.dma_start(out=xT.ap[:, i * 8:(i + 1) * 8, :], in_=src[:, i * 8:(i + 1) * 8, :]) for i in range(4)],
                      reads=[x1T], writes=[xT], n=4)
                self.load(gts, self.gates, self.gates.ap[t0:t0 + TP, :].rearrange("(a p) e -> p a e", p=128))
                self.load(acc, self.accinit, self.accinit.ap[t0:t0 + TP, :].rearrange("(a p) d -> p a d", p=128))
                for ex_ in range(NE):
                    for sl in range(6):
                        wb = wg[nw % 2]; nw += 1
                        wsrc = w_gu.ap[ex_, :, sl * 256:(sl + 1) * 256].rearrange("(c p) n -> p c n", p=128)
                        S.dma(lambda e, wb=wb, wsrc=wsrc: [e.dma_start(out=wb.ap[:, 0:16, :], in_=wsrc[:, 0:16, :]),
                                                           e.dma_start(out=wb.ap[:, 16:32, :], in_=wsrc[:, 16:32, :])],
                              reads=[w_gu], writes=[wb], q="pool", n=2)
                        for f2 in range(2):
                            ch = sl * 2 + f2
                            p = pH[nh % 4]; nh += 1
                            for kc in range(KC):
                                S.op("pe", lambda e, p=p, wb=wb, kc=kc, f2=f2: e.matmul(p.ap[:], wb.ap[:, kc, f2 * 128:(f2 + 1) * 128], xT.ap[:, kc, :],
                                                                                     start=(kc == 0), stop=(kc == KC - 1)), reads=[wb, xT], writes=[p])
                            bias = bgu.ap[:, ex_, ch:ch + 1]
                            if ch < 6:
                                a = g1[ch % 2]
                                s_ = sg[ch % 2]
                                S.op("dve", lambda e, p=p, a=a, bias=bias: e.tensor_scalar(a.ap[:], p.ap[:], bias, 7.0, ALU.add, ALU.min),
                                     reads=[p, bgu], writes=[a])
                                S.op("act", lambda e, a=a, s_=s_: e.activation(s_.ap[:], a.ap[:], AF.Sigmoid, scale=1.702), reads=[a], writes=[s_])
                                S.op("pool", lambda e, a=a, s_=s_, ch=ch: e.tensor_tensor(gluS.ap[:, ch, :], a.ap[:], s_.ap[:], ALU.mult),
                                     reads=[a, s_], writes=[gluS])
                            else:
                                j = ch - 6
                                a = l1[ch % 2]
                                S.op("dve", lambda e, p=p, a=a, bias=bias: e.tensor_scalar(a.ap[:], p.ap[:], bias, -6.0, ALU.add, ALU.max),
                                     reads=[p, bgu], writes=[a])
                                S.op("dve", lambda e, a=a, j=j: e.scalar_tensor_tensor(actT.ap[:, j, :], a.ap[:], 8.0, gluS.ap[:, j, :], ALU.min, ALU.mult),
                                     reads=[a, gluS], writes=[actT])
                    for dq in range(4):
                        wb = wd[nd % 2]; nd += 1
                        wsrc = w_dn.ap[ex_, :, dq * 1024:(dq + 1) * 1024].rearrange("(c p) n -> p c n", p=128)
                        S.dma(lambda e, wb=wb, wsrc=wsrc: [e.dma_start(out=wb.ap[:, 0:3, :], in_=wsrc[:, 0:3, :]),
                                                           e.dma_start(out=wb.ap[:, 3:6, :], in_=wsrc[:, 3:6, :])],
                              reads=[w_dn], writes=[wb], q="pool", n=2)
                        for tt in range(TP // 128):
                            for dc in range(2):
                                p = pY[ny % 4]; ny += 1
                                for j in range(6):
                                    S.op("pe", lambda e, p=p, wb=wb, j=j, tt=tt, dc=dc: e.matmul(
                                        p.ap[:], actT.ap[:, j, tt * 128:(tt + 1) * 128], wb.ap[:, j, dc * 512:(dc + 1) * 512],
                                        start=(j == 0), stop=(j == 5)), reads=[actT, wb], writes=[p])
                                cs = slice(dq * 1024 + dc * 512, dq * 1024 + (dc + 1) * 512)
                                S.op("dve", lambda e, p=p, tt=tt, cs=cs, ex_=ex_: e.scalar_tensor_tensor(
                                    acc.ap[:, tt, cs], p.ap[:], gts.ap[:, tt, ex_:ex_ + 1], acc.ap[:, tt, cs], ALU.mult, ALU.add),
                                    reads=[p, gts, acc], writes=[acc])
                for tt in range(TP // 128):
                    for dc in range(8):
                        xb = xr[(tt * 8 + dc) % 2]
                        r0 = t0 + tt * 128
                        cs = slice(dc * 512, (dc + 1) * 512)
                        S.dma(lambda e, xb=xb, r0=r0, cs=cs: e.dma_start(out=xb.ap[:], in_=x1.ap[r0:r0 + 128, cs]), reads=[x1], writes=[xb])
                        S.op("dve", lambda e, xb=xb, tt=tt, cs=cs: e.scalar_tensor_tensor(
                            xb.ap[:], xb.ap[:], ALPHA, acc.ap[:, tt, cs], ALU.mult, ALU.add), reads=[xb, acc], writes=[xb])
                        S.dma(lambda e, xb=xb, r0=r0, cs=cs: e.dma_start(out=y2.ap[r0:r0 + 128, cs], in_=xb.ap[:]), reads=[xb], writes=[y2])
            S.flush()
        return y2

    def sb_proj(self, xT_prev, xT_own, w_kv, w_q, pm=False):
        S = self.S
        self.kT = self.dscr("kT", [D, 2 * NT], BF16)
        self.vtm = self.dscr("vtm", [2 * NT, D], BF16)
        self.sqT = self.dscr("sqT", [D, NT], BF16)
        with ExitStack() as es:
            xT = self.sb(es, "xT", [128, KC, NT], BF16)
            wsl = [self.sb(es, "wsl%d" % i, [128, KC, 512], BF16) for i in range(2)]
            pss = [self.ps(es, "pp%d" % i, [128, 512]) for i in range(4)]
            stb = [self.sb(es, "stb%d" % i, [128, 512], BF16) for i in range(3)]
            self.lin_state = {"w": 0, "p": 0}
            cnt = {"b": 0}
            for (srcb, tok0, own) in ((xT_own, NT, True), (xT_prev, 0, False)):
                if pm:
                    for k4 in range(4):
                        S.dma(lambda e, k4=k4, srcb=srcb: e.dma_start(out=xT.ap[:, 8 * k4:8 * k4 + 8, :],
                                                                     in_=srcb[k4].ap[0:128, :].rearrange("p (c t) -> p c t", c=8)),
                              reads=[srcb[k4]], writes=[xT])
                else:
                    src = srcb.ap.rearrange("(c p) t -> p c t", p=128)
                    S.dma(lambda e, src=src: [e.dma_start(out=xT.ap[:, i * 8:(i + 1) * 8, :], in_=src[:, i * 8:(i + 1) * 8, :]) for i in range(4)],
                          reads=[srcb], writes=[xT], n=4)

                def ep_k(ps, f0, nf, tg, tok0=tok0):
                    sbuf = stb[cnt["b"] % 3]; cnt["b"] += 1
                    S.op("act", lambda e: e.copy(sbuf.ap[:], ps.ap[:]), reads=[ps], writes=[sbuf])
                    S.dma(lambda e: e.dma_start(out=self.kT.ap[f0:f0 + 128, tok0 + tg * 512:tok0 + (tg + 1) * 512], in_=sbuf.ap[:]),
                          reads=[sbuf], writes=[self.kT])

                def ep_v(ps, tt, c0, ncw, tok0=tok0):
                    sbuf = stb[cnt["b"] % 3]; cnt["b"] += 1
                    S.op("dve", lambda e: e.tensor_copy(sbuf.ap[:], ps.ap[:]), reads=[ps], writes=[sbuf])
                    r0 = tok0 + tt * 128
                    S.dma(lambda e: e.dma_start(out=self.vtm.ap[r0:r0 + 128, c0 - D:c0 - D + 512], in_=sbuf.ap[:]),
                          reads=[sbuf], writes=[self.vtm])

                def ep_q(ps, f0, nf, tg):
                    sbuf = stb[cnt["b"] % 3]; cnt["b"] += 1
                    S.op("act", lambda e: e.activation(sbuf.ap[:], ps.ap[:], AF.Copy, scale=SB_DH ** -0.5), reads=[ps], writes=[sbuf])
                    S.dma(lambda e: e.dma_start(out=self.sqT.ap[f0:f0 + 128, tg * 512:(tg + 1) * 512], in_=sbuf.ap[:]),
                          reads=[sbuf], writes=[self.sqT])
                self.linear(es, xT, 0, NT, w_kv, w_kv.ap, [(c, 512) for c in range(0, D, 512)], "fm", ep_k, wsl, pss)
                self.linear(es, xT, 0, NT, w_kv, w_kv.ap, [(c, 512) for c in range(D, 2 * D, 512)], "tm", ep_v, wsl, pss)
                if own:
                    self.linear(es, xT, 0, NT, w_q, w_q.ap, [(c, 512) for c in range(0, D, 512)], "fm", ep_q, wsl, pss)
            S.flush()

    def sb_attn(self):
        S = self.S
        self.oT = self.dscr("oT", [D, NT], BF16)
        with ExitStack() as es:
            sb = lambda n, s, d: self.sb(es, n, s, d)
            L = sb("L", [128, 128], F32)
            ones = sb("ones", [128, 128], F32)
            mask = sb("mask", [128, 4, 512], F32)
            hpb = sb("hpb", [128, 1], F32)
            self.load(L, self.c_L, self.c_L.ap)
            self.load(ones, self.c_ones, self.c_ones.ap)
            self.load(mask, self.c_mask, self.c_mask.ap)
            self.load(hpb, self.c_hpbias, self.c_hpbias.ap)
            kTh = [sb("kTh%d" % i, [128, 2 * NT], BF16) for i in range(2)]
            vh = [sb("vh%d" % i, [128, 16, 128], BF16) for i in range(2)]
            qTh = [sb("qTh%d" % i, [128, NT], BF16) for i in range(2)]
            ee = [sb("ee%d" % i, [128, 512], F32) for i in range(2)]
            spb = [sb("spb%d" % i, [128, 512], F32) for i in range(3)]
            accSs = [sb("accS%d" % i, [128, 512], F32) for i in range(2)]
            d1 = [sb("d1_%d" % i, [128, 512], F32) for i in range(4)]
            wf = [sb("wf%d" % i, [128, 512], F32) for i in range(2)]
            wT = [sb("wT%d" % i, [128, 512], BF16) for i in range(3)]
            osb = [sb("osb%d" % i, [128, 512], BF16) for i in range(2)]
            pZ = [self.ps(es, "pZ%d" % i, [128, 512]) for i in range(2)]
            pX = [self.ps(es, "pX%d" % i, [128, 512]) for i in range(2)]
            pXb = [self.ps(es, "pXb%d" % i, [128, 512]) for i in range(2)]
            pO = [self.ps(es, "pO%d" % i, [128, 512]) for i in range(2)]

            def head_loads(h):
                hb = h % 2
                self.load(kTh[hb], self.kT, self.kT.ap[h * 128:(h + 1) * 128, :])
                self.load(vh[hb], self.vtm, self.vtm.ap[:, h * 128:(h + 1) * 128].rearrange("(a p) d -> p a d", p=128))
                self.load(qTh[hb], self.sqT, self.sqT.ap[h * 128:(h + 1) * 128, :])

            tiles = []
            g = 0
            for h in range(SB_H):
                for qg in range(2):
                    blocks = [("own", kb) for kb in range(4 * qg + 3, -1, -1)] + [("prev", kb) for kb in range(7, -1, -1)]
                    for bi_, (kind, kb) in enumerate(blocks):
                        mj = kb - 4 * qg if (kind == "own" and kb >= 4 * qg) else None
                        tiles.append(dict(i=len(tiles), h=h, hb=h % 2, qg=qg, kind=kind, kidx=kb + (8 if kind == "own" else 0), mj=mj,
                                          first=(bi_ == 0), last=(bi_ == len(blocks) - 1), g=g,
                                          head_last=(qg == 1 and bi_ == len(blocks) - 1)))
                    g += 1

            def S1(t):
                i, hb = t["i"], t["hb"]
                pz, e_, s_, d_ = pZ[i % 2], ee[i % 2], spb[i % 3], d1[i % 4]
                kidx, qg = t["kidx"], t["qg"]
                S.op("pe", lambda e: e.matmul(pz.ap[:], kTh[hb].ap[:, kidx * 128:(kidx + 1) * 128], qTh[hb].ap[:, qg * 512:(qg + 1) * 512],
                                              start=True, stop=True), reads=[kTh[hb], qTh[hb]], writes=[pz])
                S.op("act", lambda e: e.activation(e_.ap[:], pz.ap[:], AF.Exp), reads=[pz], writes=[e_])
                S.op("act", lambda e: e.activation(s_.ap[:], e_.ap[:], AF.Ln, bias=1.0), reads=[e_], writes=[s_])

            def S1b(t):
                i = t["i"]
                pz, s_, d_ = pZ[i % 2], spb[i % 3], d1[i % 4]
                S.op("dve", lambda e: e.tensor_tensor(d_.ap[:], pz.ap[:], s_.ap[:], ALU.subtract), reads=[pz, s_], writes=[d_])

            def S2(t):
                i = t["i"]
                s_, px, py = spb[i % 3], pX[i % 2], pXb[i % 2]
                mj = t["mj"]
                if mj is not None:
                    S.op("dve", lambda e: e.tensor_tensor(s_.ap[:], s_.ap[:], mask.ap[:, mj, :], ALU.mult), reads=[s_, mask], writes=[s_])
                S.op("pe", lambda e: e.matmul(px.ap[:], L.ap[:], s_.ap[:], start=True, stop=True), reads=[L, s_], writes=[px])
                accP, accN = accSs[(i + 1) % 2], accSs[i % 2]
                if not t["first"]:
                    S.op("pe", lambda e: e.matmul(py.ap[:], ones.ap[:], accP.ap[:], start=True, stop=True), reads=[ones, accP], writes=[py])
                if not t["last"]:
                    if t["first"]:
                        S.op("pool", lambda e: e.tensor_copy(accN.ap[:], s_.ap[:]), reads=[s_], writes=[accN])
                    else:
                        S.op("dve", lambda e: e.tensor_tensor(accN.ap[:], accP.ap[:], s_.ap[:], ALU.add), reads=[s_, accP], writes=[accN])

            def S3(t):
                i, hb, h, qg = t["i"], t["hb"], t["h"], t["qg"]
                d_, px, py, w_, wt = d1[i % 4], pX[i % 2], pXb[i % 2], wf[i % 2], wT[i % 3]
                po, ob = pO[t["g"] % 2], osb[t["g"] % 2]
                mj, kidx = t["mj"], t["kidx"]
                S.op("dve", lambda e: e.tensor_tensor(d_.ap[:], d_.ap[:], px.ap[:], ALU.subtract), reads=[d_, px], writes=[d_])
                if not t["first"]:
                    S.op("dve", lambda e: e.tensor_tensor(d_.ap[:], d_.ap[:], py.ap[:], ALU.subtract), reads=[d_, py], writes=[d_])
                if mj is not None:
                    S.op("act", lambda e: e.activation(w_.ap[:], d_.ap[:], AF.Exp), reads=[d_], writes=[w_])
                    S.op("dve", lambda e: e.tensor_tensor(wt.ap[:], w_.ap[:], mask.ap[:, mj, :], ALU.mult), reads=[w_, mask], writes=[wt])
                elif t["kind"] == "prev":
                    S.op("act", lambda e: e.activation(wt.ap[:], d_.ap[:], AF.Exp, bias=hpb.ap[:]), reads=[d_, hpb], writes=[wt])
                else:
                    S.op("act", lambda e: e.activation(wt.ap[:], d_.ap[:], AF.Exp), reads=[d_], writes=[wt])

            def S3b(t):
                i, hb, h, qg = t["i"], t["hb"], t["h"], t["qg"]
                wt = wT[i % 3]
                po, ob = pO[t["g"] % 2], osb[t["g"] % 2]
                kidx = t["kidx"]
                first, last = t["first"], t["last"]
                S.op("pe", lambda e: e.matmul(po.ap[:], vh[hb].ap[:, kidx, :], wt.ap[:], start=first, stop=last), reads=[vh[hb], wt], writes=[po])
                if last:
                    S.op("act", lambda e: e.copy(ob.ap[:], po.ap[:]), reads=[po], writes=[ob])
                    S.dma(lambda e: e.dma_start(out=self.oT.ap[h * 128:(h + 1) * 128, qg * 512:(qg + 1) * 512], in_=ob.ap[:]),
                          reads=[ob], writes=[self.oT])
                if t["head_last"] and h + 2 < SB_H:
                    head_loads(h + 2)

            head_loads(0)
            head_loads(1)
            n = len(tiles)
            for step in range(n + 2):
                if step < n:
                    S1(tiles[step])
                if 0 <= step - 2 < n:
                    S3(tiles[step - 2])
                if 0 <= step - 1 < n:
                    S2(tiles[step - 1])
                if step < n:
                    S1b(tiles[step])
                if 0 <= step - 2 < n:
                    S3b(tiles[step - 2])
            S.flush()


def _declare_layer_common(P, l, big=True):
    w = {}
    w["router_w"] = P.din("router_w%d" % l, [D, NE])
    w["router_b"] = P.din("router_b%d" % l, [1, NE])
    if big:
        w["w_gu"] = P.din("w_gu%d" % l, [NE, 6, 128, KC, 256])
        w["w_dn"] = P.din("w_dn%d" % l, [NE, FF, D])
    w["b_guT"] = P.din("b_guT%d" % l, [128, NE, 12])
    w["b_dn"] = P.din("b_dn%d" % l, [NE, D])
    for nm in ("ln1_g", "ln1_b", "ln2_g", "ln2_b"):
        w[nm] = P.din("%s%d" % (nm, l), [1, D])
    return w


def build_layer0(nph=99):
    P = Prog([0])
    P.declare_consts()
    xT_all = P.din("xT_all", [D, 2 * NT])
    x_own = P.din("x_own", [NT, D])
    w_in = P.din("gla_w_in", [D, GIN])
    w_g2 = P.din("gla_w_gate2", [16, QK])
    b_g2 = P.din("gla_b_gate2", [1, QK])
    ng = P.din("gla_norm_g", [1, DV])
    w_out = P.din("gla_w_out", [GV, D])
    if nph >= 4:
        w = _declare_layer_common(P, 0, big=(nph >= 5))
    x_l0 = P.dout("x_l0", [NT, D])
    x_l0T = P.dout("x_l0T", [D, NT], BF16)
    P.gla_inproj(xT_all, w_in)
    if nph >= 2:
        P.gla_scan(w_g2, b_g2, ng)
    if nph >= 3:
        y = P.outproj_residual(P.ogT, w_out, x_own, "a0")
    if nph >= 4:
        x1 = P.dscr("x1_0", [NT, D], F32)
        x1T = P.dscr("x1T_0", [D, NT], BF16)
        import os
        dbg = int(os.environ.get("LNDBG", "2"))
        P.ln_phase(y, w["ln1_g"], w["ln1_b"], x1, x1T if dbg >= 1 else None, (w["router_w"], w["router_b"], w["b_dn"]) if dbg >= 2 else None, tag="0")
    if nph >= 5:
        y2 = P.moe(x1T, x1, w["w_gu"], w["b_guT"], w["w_dn"], "0")
    if nph >= 6:
        P.ln_phase(y2, w["ln2_g"], w["ln2_b"], x_l0, x_l0T, None, tag="0b")
    if nph < 6:
        src = {1: P.k_tm, 2: None, 3: None, 4: None, 5: None}[nph] if nph == 1 else None
        with ExitStack() as es:
            t = P.sb(es, "dbg", [128, D], F32)
            tb = P.sb(es, "dbgb", [128, D], BF16)
            for tt in range(8):
                if nph == 1:
                    P.S.dma(lambda e, tt=tt: e.dma_start(out=t.ap[:, 0:QK], in_=P.k_tm.ap[NT + tt * 128:NT + (tt + 1) * 128, :]), reads=[P.k_tm], writes=[t])
                elif nph == 2:
                    P.S.dma(lambda e, tt=tt: e.dma_start(out=tb.ap[:, 0:NT], in_=P.ogT.ap[tt * 128:(tt + 1) * 128, :]), reads=[P.ogT], writes=[tb])
                    P.S.op("dve", lambda e: e.tensor_copy(t.ap[:, 0:NT], tb.ap[:, 0:NT]), reads=[tb], writes=[t])
                elif nph == 3:
                    P.S.dma(lambda e, tt=tt: e.dma_start(out=t.ap[:], in_=y.ap[tt * 128:(tt + 1) * 128, :]), reads=[y], writes=[t])
                elif nph == 4:
                    P.S.dma(lambda e, tt=tt: e.dma_start(out=t.ap[:], in_=x1.ap[tt * 128:(tt + 1) * 128, :]), reads=[x1], writes=[t])
                elif nph == 5:
                    P.S.dma(lambda e, tt=tt: e.dma_start(out=t.ap[:], in_=y2.ap[tt * 128:(tt + 1) * 128, :]), reads=[y2], writes=[t])
                P.S.dma(lambda e, tt=tt: e.dma_start(out=x_l0.ap[tt * 128:(tt + 1) * 128, :], in_=t.ap[:]), reads=[t], writes=[x_l0])
            P.S.flush()
    P.S.close()
    return P


def build_layer1():
    P = Prog([1])
    P.declare_consts()
    xT_prev = P.din("xT_prev", [D, NT], BF16)
    xT_own = P.din("xT_own", [D, NT], BF16)
    x_own = P.din("x_own", [NT, D])
    w_kv = P.din("shared_w_kv", [D, 2 * D])
    w_q = P.din("sb_w_q", [D, D])
    w_out = P.din("sb_w_out", [D, D])
    w = _declare_layer_common(P, 1)
    out = P.dout("out", [NT, D])
    P.sb_proj(xT_prev, xT_own, w_kv, w_q)
    P.sb_attn()
    y = P.outproj_residual(P.oT, w_out, x_own, "a1")
    x1 = P.dscr("x1_1", [NT, D], F32)
    x1T = P.dscr("x1T_1", [D, NT], BF16)
    P.ln_phase(y, w["ln1_g"], w["ln1_b"], x1, x1T, (w["router_w"], w["router_b"], w["b_dn"]), tag="1")
    y2 = P.moe(x1T, x1, w["w_gu"], w["b_guT"], w["w_dn"], "1")
    P.ln_phase(y2, w["ln2_g"], w["ln2_b"], out, None, None, tag="1b")
    P.S.close()
    return P


def _consts(h):
    i = np.arange(128)
    c = {}
    c["c_ident"] = np.eye(128, dtype=np.float32)
    c["c_identb"] = np.eye(128, dtype=np.float32).astype(ml_dtypes.bfloat16)
    c["c_U"] = ((i[:, None] > i[None, :]) & ((i[:, None] // 64) == (i[None, :] // 64))).astype(np.float32)
    c["c_ind"] = np.stack([(i // 64 == 0), (i // 64 == 1)], axis=1).astype(np.float32)
    c["c_L"] = (i[:, None] > i[None, :]).astype(np.float32)
    c["c_ones"] = np.ones((128, 128), np.float32)
    m = np.zeros((128, 4, 512), np.float32)
    tq = np.arange(512)
    for j in range(4):
        jb = tq // 128
        tri = (i[:, None] < (tq % 128)[None, :])
        m[:, j, :] = np.where(jb[None, :] < j, 0.0, np.where(jb[None, :] == j, tri, 1.0))
    c["c_mask"] = m
    c["c_hpbias"] = np.full((128, 1), 0.0 if h == 1 else -30000.0, np.float32)
    return c


def _layer_common_inputs(l, inp):
    d = {}
    d["router_w%d" % l] = np.ascontiguousarray(inp["router_w"][l])
    d["router_b%d" % l] = np.ascontiguousarray(inp["router_b"][l][None, :])
    d["w_gu%d" % l] = np.ascontiguousarray(inp["moe_w_gate_up"][l].reshape(NE, KC, 128, 6, 256).transpose(0, 3, 2, 1, 4))
    bgu = inp["moe_b_gate_up"][l]
    d["b_guT%d" % l] = np.ascontiguousarray(bgu.reshape(NE, 12, 128).transpose(2, 0, 1))
    d["w_dn%d" % l] = np.ascontiguousarray(inp["moe_w_down"][l])
    d["b_dn%d" % l] = np.ascontiguousarray(inp["moe_b_down"][l])
    for nm in ("ln1_g", "ln1_b", "ln2_g", "ln2_b"):
        d["%s%d" % (nm, l)] = np.ascontiguousarray(inp[nm][l][None, :])
    return d


def build_fused():
    P = Prog([0, 1])
    P.declare_consts()
    xT_all = P.din("xT_all", [D, 2 * NT])
    x_own = P.din("x_own", [NT, D])
    w_in = P.din("gla_w_in", [D, GIN])
    w_g2 = P.din("gla_w_gate2", [16, QK])
    b_g2 = P.din("gla_b_gate2", [1, QK])
    ng = P.din("gla_norm_g", [1, DV])
    w_out0 = P.din("gla_w_out", [GV, D])
    w0 = _declare_layer_common(P, 0)
    w_kv = P.din("shared_w_kv", [D, 2 * D])
    w_q = P.din("sb_w_q", [D, D])
    w_out1 = P.din("sb_w_out", [D, D])
    w1 = _declare_layer_common(P, 1)
    out = P.dout("out", [NT, D])
    S = P.S
    P.gla_inproj(xT_all, w_in)
    P.gla_scan(w_g2, b_g2, ng)
    y = P.outproj_residual(P.ogT, w_out0, x_own, "a0")
    x1 = P.dscr("x1_0", [NT, D], F32)
    x1T = P.dscr("x1T_0", [D, NT], BF16)
    P.ln_phase(y, w0["ln1_g"], w0["ln1_b"], x1, x1T, (w0["router_w"], w0["router_b"], w0["b_dn"]), tag="0")
    y2 = P.moe(x1T, x1, w0["w_gu"], w0["b_guT"], w0["w_dn"], "0")
    x_l0 = P.dscr("x_l0", [NT, D], F32)
    src = [P.dscr("xsrc%d" % k, [128, 8192], BF16) for k in range(4)]
    dst = [P.dscr("xdst%d" % k, [256, 8192], BF16) for k in range(4)]
    P.ln_phase(y2, w0["ln2_g"], w0["ln2_b"], x_l0, None, None, tag="0b", xT_pm=src)
    rg = [[0, 1], [2, 3], [4, 5], [6, 7]]
    for k in range(4):
        S.dma(lambda e, k=k: e.collective_compute("AllGather", ALU.bypass, replica_groups=rg,
                                                  ins=[src[k].ap.opt()], outs=[dst[k].ap.opt()]),
              reads=[src[k]], writes=[dst[k]], q="pool", amt=1)
    S.flush()
    P.sb_proj(dst, src, w_kv, w_q, pm=True)
    P.sb_attn()
    y = P.outproj_residual(P.oT, w_out1, x_l0, "a1")
    x1b = P.dscr("x1_1", [NT, D], F32)
    x1Tb = P.dscr("x1T_1", [D, NT], BF16)
    P.ln_phase(y, w1["ln1_g"], w1["ln1_b"], x1b, x1Tb, (w1["router_w"], w1["router_b"], w1["b_dn"]), tag="1")
    y2 = P.moe(x1Tb, x1b, w1["w_gu"], w1["b_guT"], w1["w_dn"], "1")
    P.ln_phase(y2, w1["ln2_g"], w1["ln2_b"], out, None, None, tag="1b")
    P.S.close()
    return P


_CACHE = {}


def kernel(**inp):
    inp = {k: np.asarray(v) for k, v in inp.items()}
    x = inp["x"]
    B = x.shape[0]
    ncore = 2 * B
    if "pf" not in _CACHE:
        _CACHE["pf"] = build_fused()
    P = _CACHE["pf"]
    com = _layer_common_inputs(0, inp)
    com.update(_layer_common_inputs(1, inp))
    com.update({"gla_w_in": np.ascontiguousarray(inp["gla_w_in"][0]), "gla_w_gate2": np.ascontiguousarray(inp["gla_w_gate2"][0]),
                "gla_b_gate2": np.ascontiguousarray(inp["gla_b_gate2"][0][None, :]), "gla_norm_g": np.ascontiguousarray(inp["gla_norm_g"][0][None, :]),
                "gla_w_out": np.ascontiguousarray(inp["gla_w_out"][0]),
                "shared_w_kv": np.ascontiguousarray(inp["shared_w_kv"]), "sb_w_q": np.ascontiguousarray(inp["sb_w_q"][0]),
                "sb_w_out": np.ascontiguousarray(inp["sb_w_out"][0])})
    maps = []
    for c in range(ncore):
        b, h = c // 2, c % 2
        m = dict(com)
        m.update(_consts(h))
        xb = x[b]
        xT_all = np.zeros((D, 2 * NT), np.float32)
        xT_all[:, NT:] = xb[h * NT:(h + 1) * NT].T
        if h == 1:
            xT_all[:, :NT] = xb[:NT].T
        m["xT_all"] = xT_all
        m["x_own"] = np.ascontiguousarray(xb[h * NT:(h + 1) * NT])
        maps.append(m)
    r = run_bass_kernel_spmd(P.nc, maps, core_ids=list(range(ncore))).results
    out = np.zeros((B, 2 * NT, D), np.float32)
    for c in range(ncore):
        b, h = c // 2, c % 2
        out[b, h * NT:(h + 1) * NT] = r[c]["out"]
    return out
```

```python
import numpy as np
import ml_dtypes
from contextlib import ExitStack
import concourse.bass as bass
import concourse.mybir as mybir
from concourse.bass_utils import run_bass_kernel_spmd

F32 = mybir.dt.float32
BF16 = mybir.dt.bfloat16
ALU = mybir.AluOpType
AF = mybir.ActivationFunctionType

D = 4096
KC = 32
NT = 1024
NH_GLA = 8
DK = 256
DV = 512
QK = 2048
GV = 4096
GIN = 12304
TAU = 16.0
NE = 32
FF = 768
DEPTH = 2
ALPHA = (2.0 * DEPTH) ** 0.25
LN_EPS = 1e-5
RMS_EPS = 1e-6
SB_H = 32
SB_DH = 128


class Buf:
    __slots__ = ("name", "ap", "last_w", "reads", "dsem")

    def __init__(self, name, ap=None):
        self.name = name
        self.ap = ap
        self.last_w = None
        self.reads = {}
        self.dsem = None

    def __getitem__(self, idx):
        return self.ap[idx]


class Sched:
    ENG = ("sp", "pe", "act", "dve", "pool")

    def __init__(self, nc):
        self.nc = nc
        self.streams = {e: [] for e in self.ENG}
        self.count = {}
        self.seen = {e: {} for e in self.ENG}
        self.sem_keys = []
        self.sems = {}
        self.es = ExitStack()
        self.free_keys = []
        self.dsem_bufs = []
        for e in ("pe", "act", "dve", "pool"):
            self._new_sem("E_" + e)

    def _new_sem(self, key):
        self.sem_keys.append(key)
        self.count[key] = 0
        return key

    def _waits(self, eng, reads, writes):
        need = {}

        def add(sig):
            if sig is None:
                return
            k, v = sig
            if need.get(k, 0) < v:
                need[k] = v
        for b in reads:
            add(b.last_w)
        for b in writes:
            add(b.last_w)
            for k, v in b.reads.items():
                add((k, v))
        out = []
        seen = self.seen[eng]
        for k, v in need.items():
            if k == "E_pe" and eng == "pe":
                continue
            if seen.get(k, 0) >= v:
                continue
            seen[k] = v
            out.append((k, v))
        return out

    def _mark(self, sig, reads, writes):
        key, val = sig
        for b in reads:
            if b.reads.get(key, 0) < val:
                b.reads[key] = val
        for b in writes:
            b.last_w = sig
            b.reads = {}

    def op(self, eng, fn, reads=(), writes=()):
        waits = self._waits(eng, reads, writes)
        key = "E_" + eng
        self.count[key] += 1
        sig = (key, self.count[key])
        self.streams[eng].append((waits, fn, key, 1, 1))
        self._mark(sig, reads, writes)
        return sig

    def dma(self, fn, reads=(), writes=(), q="sp", n=1, amt=16):
        dst = writes[0]
        if dst.dsem is None:
            if self.free_keys:
                dst.dsem = self.free_keys.pop()
            else:
                dst.dsem = self._new_sem("D%d" % len(self.sem_keys))
            self.dsem_bufs.append(dst)
        key = dst.dsem
        waits = self._waits(q, reads, writes)
        self.count[key] += amt * n
        sig = (key, self.count[key])
        self.streams[q].append((waits, fn, key, amt, n))
        self._mark(sig, reads, writes)
        return sig

    def flush(self):
        nc = self.nc
        for e in self.ENG:
            waits = []
            for k in self.sem_keys:
                v = self.count[k]
                if v > 0 and self.seen[e].get(k, 0) < v:
                    self.seen[e][k] = v
                    waits.append((k, v))
            self.streams[e].append((waits, None, None, 0, 0))
        for k in self.sem_keys:
            if k not in self.sems and self.count[k] > 0:
                self.sems[k] = self.es.enter_context(nc.semaphore(k))
        sems = self.sems

        def run(stream):
            def body(eng):
                for waits, fn, key, amt, n in stream:
                    for (k, v) in waits:
                        eng.wait_ge(sems[k], v)
                    if fn is None:
                        continue
                    r = fn(eng)
                    if not isinstance(r, (list, tuple)):
                        r = [r]
                    assert len(r) == n, (len(r), n)
                    for ins in r:
                        ins.then_inc(sems[key], amt)
            return body
        with nc.Block() as block:
            block.sync(run(self.streams["sp"]))
            block.tensor(run(self.streams["pe"]))
            block.scalar(run(self.streams["act"]))
            block.vector(run(self.streams["dve"]))
            block.gpsimd(run(self.streams["pool"]))
        self.streams = {e: [] for e in self.ENG}
        for b in self.dsem_bufs:
            self.free_keys.append(b.dsem)
            b.dsem = None
        self.dsem_bufs = []

    def close(self):
        self.es.close()


class Prog:
    def __init__(self, layers):
        self.layers = layers
        self.nc = bass.Bass("TRN2", target_bir_lowering=False)
        self.S = Sched(self.nc)
        self.dram = {}
        self.out_names = []

    def din(self, name, shape, dt=F32):
        t = self.nc.dram_tensor(name, list(shape), dt, kind="ExternalInput").ap()
        self.dram[name] = Buf(name, t)
        return self.dram[name]

    def dout(self, name, shape, dt=F32):
        t = self.nc.dram_tensor(name, list(shape), dt, kind="ExternalOutput").ap()
        self.dram[name] = Buf(name, t)
        self.out_names.append(name)
        return self.dram[name]

    def dscr(self, name, shape, dt):
        t = self.nc.dram_tensor(name, list(shape), dt).ap()
        self.dram[name] = Buf(name, t)
        return self.dram[name]

    def sb(self, es, name, shape, dt):
        self.uid = getattr(self, "uid", 0) + 1
        name = "%s_u%d" % (name, self.uid)
        return Buf(name, es.enter_context(self.nc.sbuf_tensor(name, list(shape), dt)))

    def ps(self, es, name, shape, dt=F32):
        self.uid = getattr(self, "uid", 0) + 1
        name = "%s_u%d" % (name, self.uid)
        return Buf(name, es.enter_context(self.nc.psum_tensor(name, list(shape), dt)))

    def declare_consts(self):
        self.c_ident = self.din("c_ident", [128, 128])
        self.c_identb = self.din("c_identb", [128, 128], BF16)
        self.c_U = self.din("c_U", [128, 128])
        self.c_ind = self.din("c_ind", [128, 2])
        self.c_L = self.din("c_L", [128, 128])
        self.c_ones = self.din("c_ones", [128, 128])
        self.c_mask = self.din("c_mask", [128, 4, 512])
        self.c_hpbias = self.din("c_hpbias", [128, 1])

    def load(self, dst, src_buf, src_ap, q="sp"):
        self.S.dma(lambda e: e.dma_start(out=dst.ap[:], in_=src_ap), reads=[src_buf], writes=[dst], q=q)

    def linear(self, es, xT, t0, T, wbuf, w_ap, cols, mode, epilogue, wslabs, pss, sw=512):
        S = self.S
        st = self.lin_state
        for (c0, ncw) in cols:
            wb = wslabs[st["w"] % len(wslabs)]
            st["w"] += 1
            src = w_ap[:, c0:c0 + ncw].rearrange("(c p) n -> p c n", p=128)
            half = KC // 2
            S.dma(lambda e, wb=wb, src=src, ncw=ncw: [
                e.dma_start(out=wb.ap[:, 0:half, 0:ncw], in_=src[:, 0:half, :]),
                e.dma_start(out=wb.ap[:, half:KC, 0:ncw], in_=src[:, half:KC, :])],
                reads=[wbuf], writes=[wb], q="pool", n=2)
            if mode == "tm":
                for tt in range(T // 128):
                    ps = pss[st["p"] % len(pss)]
                    st["p"] += 1
                    for kc in range(KC):
                        S.op("pe", lambda e, ps=ps, wb=wb, kc=kc, tt=tt, ncw=ncw: e.matmul(
                            ps.ap[:, 0:ncw], xT.ap[:, kc, t0 + tt * 128:t0 + (tt + 1) * 128], wb.ap[:, kc, 0:ncw],
                            start=(kc == 0), stop=(kc == KC - 1)), reads=[xT, wb], writes=[ps])
                    epilogue(ps, tt, c0, ncw)
            else:
                ntg = T // 512
                for fc in range((ncw + 127) // 128):
                    nf = min(128, ncw - fc * 128)
                    pl = []
                    for tg in range(ntg):
                        pl.append(pss[st["p"] % len(pss)])
                        st["p"] += 1
                    for kc in range(KC):
                        for tg in range(ntg):
                            ps = pl[tg]
                            S.op("pe", lambda e, ps=ps, wb=wb, kc=kc, tg=tg, fc=fc, nf=nf: e.matmul(
                                ps.ap[0:nf, 0:512], wb.ap[:, kc, fc * 128:fc * 128 + nf],
                                xT.ap[:, kc, t0 + tg * 512:t0 + (tg + 1) * 512],
                                start=(kc == 0), stop=(kc == KC - 1)), reads=[xT, wb], writes=[ps])
                    for tg in range(ntg):
                        epilogue(pl[tg], c0 + fc * 128, nf, tg)

    def gla_inproj(self, xT_all, w_in):
        S = self.S
        nc = self.nc
        self.qT = self.dscr("qT", [QK, NT], BF16)
        self.k_tm = self.dscr("k_tm", [2 * NT, QK], F32)
        self.v_tm = self.dscr("v_tm", [2 * NT, GV], BF16)
        self.r_tm = self.dscr("r_tm", [NT, GV], F32)
        self.gT = self.dscr("gT", [16, 2 * NT], F32)
        w_ap = w_in.ap
        with ExitStack() as es:
            xT = self.sb(es, "xT", [128, KC, NT], BF16)
            wsl = [self.sb(es, "wsl%d" % i, [128, KC, 512], BF16) for i in range(2)]
            pss = [self.ps(es, "pp%d" % i, [128, 512]) for i in range(4)]
            stf = [self.sb(es, "stf%d" % i, [128, 512], F32) for i in range(3)]
            stb = [self.sb(es, "stb%d" % i, [128, 512], BF16) for i in range(3)]
            self.lin_state = {"w": 0, "p": 0}
            cnt = {"f": 0, "b": 0}

            for (tok0, own) in ((NT, True), (0, False)):
                src = xT_all.ap[:, tok0:tok0 + NT].rearrange("(c p) t -> p c t", p=128)
                S.dma(lambda e, src=src: [e.dma_start(out=xT.ap[:, i * 8:(i + 1) * 8, :], in_=src[:, i * 8:(i + 1) * 8, :])
                                          for i in range(4)], reads=[xT_all], writes=[xT], q="pool", n=4)

                def ep_q(ps, f0, nf, tg):
                    sbuf = stb[cnt["b"] % 3]; cnt["b"] += 1
                    S.op("act", lambda e: e.activation(sbuf.ap[:], ps.ap[:], AF.Copy, scale=DK ** -0.5), reads=[ps], writes=[sbuf])
                    S.dma(lambda e: e.dma_start(out=self.qT.ap[f0:f0 + 128, tg * 512:(tg + 1) * 512], in_=sbuf.ap[:]),
                          reads=[sbuf], writes=[self.qT])

                def ep_k(ps, tt, c0, ncw, tok0=tok0):
                    sbuf = stf[cnt["f"] % 3]; cnt["f"] += 1
                    S.op("dve", lambda e: e.tensor_copy(sbuf.ap[:], ps.ap[:]), reads=[ps], writes=[sbuf])
                    r0 = tok0 + tt * 128
                    S.dma(lambda e: e.dma_start(out=self.k_tm.ap[r0:r0 + 128, c0 - QK:c0 - QK + 512], in_=sbuf.ap[:]),
                          reads=[sbuf], writes=[self.k_tm])

                def ep_v(ps, tt, c0, ncw, tok0=tok0):
                    sbuf = stb[cnt["b"] % 3]; cnt["b"] += 1
                    S.op("act", lambda e: e.copy(sbuf.ap[:], ps.ap[:]), reads=[ps], writes=[sbuf])
                    r0 = tok0 + tt * 128
                    S.dma(lambda e: e.dma_start(out=self.v_tm.ap[r0:r0 + 128, c0 - 2 * QK:c0 - 2 * QK + 512], in_=sbuf.ap[:]),
                          reads=[sbuf], writes=[self.v_tm])

                def ep_r(ps, tt, c0, ncw):
                    sbuf = stf[cnt["f"] % 3]; cnt["f"] += 1
                    S.op("act", lambda e: e.activation(sbuf.ap[:], ps.ap[:], AF.Silu), reads=[ps], writes=[sbuf])
                    r0 = tt * 128
                    cc = c0 - 2 * QK - GV
                    S.dma(lambda e: e.dma_start(out=self.r_tm.ap[r0:r0 + 128, cc:cc + 512], in_=sbuf.ap[:]),
                          reads=[sbuf], writes=[self.r_tm])

                def ep_g(ps, f0, nf, tg, tok0=tok0):
                    sbuf = stf[cnt["f"] % 3]; cnt["f"] += 1
                    S.op("dve", lambda e: e.tensor_copy(sbuf.ap[0:16, :], ps.ap[0:16, :]), reads=[ps], writes=[sbuf])
                    S.dma(lambda e: e.dma_start(out=self.gT.ap[:, tok0 + tg * 512:tok0 + (tg + 1) * 512], in_=sbuf.ap[0:16, :]),
                          reads=[sbuf], writes=[self.gT])

                if own:
                    self.linear(es, xT, 0, NT, w_in, w_ap, [(c, 512) for c in range(0, QK, 512)], "fm", ep_q, wsl, pss)
                self.linear(es, xT, 0, NT, w_in, w_ap, [(c, 512) for c in range(QK, 2 * QK, 512)], "tm", ep_k, wsl, pss)
                self.linear(es, xT, 0, NT, w_in, w_ap, [(c, 512) for c in range(2 * QK, 2 * QK + GV, 512)], "tm", ep_v, wsl, pss)
                if own:
                    self.linear(es, xT, 0, NT, w_in, w_ap, [(c, 512) for c in range(2 * QK + GV, 2 * QK + 2 * GV, 512)], "tm", ep_r, wsl, pss)
                self.linear(es, xT, 0, NT, w_in, w_ap, [(2 * QK + 2 * GV, 16)], "fm", ep_g, wsl, pss)
            S.flush()

    def gla_scan(self, w_gate2, b_gate2, norm_g):
        S = self.S
        self.ogT = self.dscr("ogT", [GV, NT], BF16)
        with ExitStack() as es:
            sb = lambda n, s, d: self.sb(es, n, s, d)
            w2 = sb("w2", [16, QK], F32)
            b2bc = sb("b2bc", [128, QK], F32)
            gbc = sb("gbc", [128, DV], F32)
            U = sb("U", [128, 128], F32)
            ind = sb("ind", [128, 2], F32)
            identb = sb("identb", [128, 128], BF16)
            self.load(w2, w_gate2, w_gate2.ap)
            self.load(b2bc, b_gate2, b_gate2.ap.partition_broadcast(128))
            self.load(gbc, norm_g, norm_g.ap.partition_broadcast(128))
            self.load(U, self.c_U, self.c_U.ap)
            self.load(ind, self.c_ind, self.c_ind.ap)
            self.load(identb, self.c_identb, self.c_identb.ap)
            Sst = [sb("Sst%d" % i, [128, DV], F32) for i in range(16)]
            Sbf = [[sb("Sbf%d_%d" % (c, i), [128, DV], BF16) for i in range(16)] for c in range(2)]
            for i in range(16):
                S.op("pool", lambda e, i=i: e.memset(Sst[i].ap[:], 0.0), writes=[Sst[i]])
            NB = 2
            gt = [sb("gt%d" % i, [16, 128], F32) for i in range(NB)]
            kt = [sb("kt%d" % i, [128, QK], F32) for i in range(NB)]
            vt = [sb("vt%d" % i, [128, GV], BF16) for i in range(NB)]
            rt = [sb("rt0", [128, GV], F32)] * NB
            Q0 = [sb("Q0_%d" % i, [128, 16, 128], BF16) for i in range(NB)]
            Q1 = [sb("Q1_%d" % i, [128, 16, 128], BF16) for i in range(NB)]
            for i in range(NB):
                S.op("pool", lambda e, i=i: e.memset(Q0[i].ap[:], 0.0), writes=[Q0[i]])
                S.op("pool", lambda e, i=i: e.memset(Q1[i].ap[:], 0.0), writes=[Q1[i]])
            sp = sb("sp", [128, QK], F32)
            kdec = sb("kdec", [128, QK], BF16)
            t1 = [sb("t1_%d" % i, [128, 512], F32) for i in range(2)]
            t2 = [sb("t2_%d" % i, [128, 512], F32) for i in range(2)]
            dec = sb("dec", [128, 32], F32)
            og = sb("og", [128, GV], BF16)
            ogTs = sb("ogTs", [128, KC, 128], BF16)
            junk = sb("junk", [128, 512], F32)
            t3 = [sb("t3_%d" % i, [128, 512], F32) for i in range(2)]
            ssq = [sb("ssq%d" % i, [128, 1], F32) for i in range(2)]
            rstd = [sb("rstd%d" % i, [128, 1], F32) for i in range(2)]
            pA = [self.ps(es, "pA%d" % i, [128, 512]) for i in range(2)]
            pD = self.ps(es, "pD", [128, 32])
            pS = [self.ps(es, "pS%d" % i, [128, 512]) for i in range(2)]
            pO = [self.ps(es, "pO%d" % i, [128, 512]) for i in range(2)]
            pT = self.ps(es, "pT", [128, 512], BF16)
            nS = 0
            nO = 0
            for ti in range(16):
                own = ti >= 8
                bi = ti % NB
                r0 = ti * 128
                self.load(gt[bi], self.gT, self.gT.ap[:, r0:r0 + 128])
                self.load(kt[bi], self.k_tm, self.k_tm.ap[r0:r0 + 128, :])
                self.load(vt[bi], self.v_tm, self.v_tm.ap[r0:r0 + 128, :])
                if own:
                    o0 = r0 - NT
                    self.load(rt[bi], self.r_tm, self.r_tm.ap[o0:o0 + 128, :])
                    qsrc = self.qT.ap.rearrange("(c p) t -> p c t", p=128)
                    S.dma(lambda e, bi=bi, o0=o0, qsrc=qsrc: e.dma_start(out=Q0[bi].ap[:, :, 0:64], in_=qsrc[:, :, o0:o0 + 64]),
                          reads=[self.qT], writes=[Q0[bi]])
                    S.dma(lambda e, bi=bi, o0=o0, qsrc=qsrc: e.dma_start(out=Q1[bi].ap[:, :, 64:128], in_=qsrc[:, :, o0 + 64:o0 + 128]),
                          reads=[self.qT], writes=[Q1[bi]])
                for blk in range(4):
                    cs = slice(blk * 512, (blk + 1) * 512)
                    pz = pA[0]
                    pr = pA[1]
                    a1 = t1[blk % 2]
                    a2 = t2[blk % 2]
                    S.op("pe", lambda e, pz=pz, bi=bi, cs=cs: e.matmul(pz.ap[:], gt[bi].ap[:], w2.ap[:, cs], start=True, stop=True),
                         reads=[gt[bi], w2], writes=[pz])
                    S.op("dve", lambda e, pz=pz, a1=a1, cs=cs: e.tensor_tensor(a1.ap[:], pz.ap[:], b2bc.ap[:, cs], ALU.add),
                         reads=[pz, b2bc], writes=[a1])
                    S.op("act", lambda e, a1=a1: e.activation(a1.ap[:], a1.ap[:], AF.Exp, scale=-1.0), reads=[a1], writes=[a1])
                    S.op("act", lambda e, a1=a1, cs=cs: e.activation(sp.ap[:, cs], a1.ap[:], AF.Ln, bias=1.0), reads=[a1], writes=[sp])
                    S.op("pe", lambda e, pr=pr, cs=cs: e.matmul(pr.ap[:], U.ap[:], sp.ap[:, cs], start=True, stop=True),
                         reads=[U, sp], writes=[pr])
                    S.op("act", lambda e, pr=pr, a2=a2: e.activation(a2.ap[:], pr.ap[:], AF.Exp, scale=-1.0 / TAU), reads=[pr], writes=[a2])
                    S.op("dve", lambda e, a2=a2, bi=bi, cs=cs: e.tensor_tensor(kdec.ap[:, cs], kt[bi].ap[:, cs], a2.ap[:], ALU.mult),
                         reads=[kt[bi], a2], writes=[kdec])
                    for j in range(4):
                        hk = blk * 4 + j
                        S.op("pe", lambda e, hk=hk: e.matmul(pD.ap[:, 2 * hk:2 * hk + 2], sp.ap[:, hk * 128:(hk + 1) * 128], ind.ap[:],
                                                             start=True, stop=True), reads=[sp, ind], writes=[pD])
                S.op("act", lambda e: e.activation(dec.ap[:], pD.ap[:], AF.Exp, scale=-1.0 / TAU), reads=[pD], writes=[dec])
                for c in range(2):
                    rs = slice(c * 64, (c + 1) * 64)
                    for hk in range(16):
                        h = hk // 2
                        pst = pS[nS % 2]; nS += 1
                        S.op("pe", lambda e, pst=pst, rs=rs, hk=hk, h=h, bi=bi: e.matmul(
                            pst.ap[:], kdec.ap[rs, hk * 128:(hk + 1) * 128], vt[bi].ap[rs, h * DV:(h + 1) * DV], start=True, stop=True),
                            reads=[kdec, vt[bi]], writes=[pst])
                        S.op("dve", lambda e, pst=pst, hk=hk, c=c: e.scalar_tensor_tensor(
                            Sst[hk].ap[:], Sst[hk].ap[:], dec.ap[:, 2 * hk + c:2 * hk + c + 1], pst.ap[:], ALU.mult, ALU.add),
                            reads=[Sst[hk], dec, pst], writes=[Sst[hk]])
                        if own:
                            S.op("act", lambda e, hk=hk, c=c: e.copy(Sbf[c][hk].ap[:], Sst[hk].ap[:]), reads=[Sst[hk]], writes=[Sbf[c][hk]])
                if not own:
                    continue
                for h in range(NH_GLA):
                    po = pO[nO % 2]
                    ui = nO % 2
                    nO += 1
                    k = 0
                    for c in range(2):
                        Q = (Q0 if c == 0 else Q1)[bi]
                        for kh in range(2):
                            hk = 2 * h + kh
                            S.op("pe", lambda e, po=po, Q=Q, hk=hk, c=c, k=k: e.matmul(
                                po.ap[:], Q.ap[:, hk, :], Sbf[c][hk].ap[:], start=(k == 0), stop=(k == 3)),
                                reads=[Q, Sbf[c][hk]], writes=[po])
                            k += 1
                    S.op("act", lambda e, po=po, ui=ui: e.activation(junk.ap[:], po.ap[:], AF.Square, accum_out=ssq[ui].ap[:]),
                         reads=[po], writes=[junk, ssq[ui]])
                    S.op("dve", lambda e, ui=ui: e.tensor_scalar(rstd[ui].ap[:], ssq[ui].ap[:], 1.0 / DV, RMS_EPS, ALU.mult, ALU.add),
                         reads=[ssq[ui]], writes=[rstd[ui]])
                    S.op("act", lambda e, ui=ui: e.activation(rstd[ui].ap[:], rstd[ui].ap[:], AF.Sqrt), reads=[rstd[ui]], writes=[rstd[ui]])
                    S.op("dve", lambda e, ui=ui: e.reciprocal(rstd[ui].ap[:], rstd[ui].ap[:]), reads=[rstd[ui]], writes=[rstd[ui]])
                    S.op("dve", lambda e, po=po, ui=ui: e.scalar_tensor_tensor(
                        t3[ui].ap[:], po.ap[:], rstd[ui].ap[:], gbc.ap[:], ALU.mult, ALU.mult), reads=[po, rstd[ui], gbc], writes=[t3[ui]])
                    S.op("pool", lambda e, ui=ui, h=h, bi=bi: e.tensor_tensor(
                        og.ap[:, h * DV:(h + 1) * DV], t3[ui].ap[:], rt[bi].ap[:, h * DV:(h + 1) * DV], ALU.mult),
                        reads=[t3[ui], rt[bi]], writes=[og])
                for g4 in range(8):
                    for j in range(4):
                        fc = g4 * 4 + j
                        S.op("pe", lambda e, fc=fc, j=j: e.transpose(pT.ap[:, j * 128:(j + 1) * 128], og.ap[:, fc * 128:(fc + 1) * 128], identb.ap[:]),
                             reads=[og, identb], writes=[pT])
                    S.op("act", lambda e, g4=g4: e.copy(ogTs.ap[:, g4 * 4:(g4 + 1) * 4, :], pT.ap[:].rearrange("p (a t) -> p a t", a=4)),
                         reads=[pT], writes=[ogTs])
                o0 = r0 - NT
                S.dma(lambda e, o0=o0: e.dma_start(out=self.ogT.ap.rearrange("(c p) t -> p c t", p=128)[:, :, o0:o0 + 128], in_=ogTs.ap[:]),
                      reads=[ogTs], writes=[self.ogT])
            S.flush()

    def outproj_residual(self, aT_dram, w, x_tm, tag):
        S = self.S
        y = self.dscr("y_" + tag, [NT, D], F32)
        with ExitStack() as es:
            xT = self.sb(es, "xT", [128, KC, NT], BF16)
            wsl = [self.sb(es, "wsl%d" % i, [128, KC, 512], BF16) for i in range(2)]
            pss = [self.ps(es, "pp%d" % i, [128, 512]) for i in range(4)]
            xs = [self.sb(es, "xs%d" % i, [128, 512], F32) for i in range(3)]
            self.lin_state = {"w": 0, "p": 0}
            cnt = {"x": 0}
            src = aT_dram.ap.rearrange("(c p) t -> p c t", p=128)
            S.dma(lambda e: [e.dma_start(out=xT.ap[:, i * 8:(i + 1) * 8, :], in_=src[:, i * 8:(i + 1) * 8, :]) for i in range(4)],
                  reads=[aT_dram], writes=[xT], n=4)

            def ep(ps, tt, c0, ncw):
                xb = xs[cnt["x"] % 3]; cnt["x"] += 1
                S.dma(lambda e: e.dma_start(out=xb.ap[:], in_=x_tm.ap[tt * 128:(tt + 1) * 128, c0:c0 + 512]), reads=[x_tm], writes=[xb])
                S.op("dve", lambda e: e.scalar_tensor_tensor(xb.ap[:], xb.ap[:], ALPHA, ps.ap[:], ALU.mult, ALU.add), reads=[xb, ps], writes=[xb])
                S.dma(lambda e: e.dma_start(out=y.ap[tt * 128:(tt + 1) * 128, c0:c0 + 512], in_=xb.ap[:]), reads=[xb], writes=[y])
            self.linear(es, xT, 0, NT, w, w.ap, [(c, 512) for c in range(0, D, 512)], "tm", ep, wsl, pss)
            S.flush()
        return y

    def ln_phase(self, y, g, b, x_out, xT_out=None, router=None, tag="", xT_pm=None):
        S = self.S
        if router is not None:
            w_r, b_r, b_dn = router
            self.gates = self.dscr("gates_" + tag, [NT, NE], F32)
            self.accinit = self.dscr("accinit_" + tag, [NT, D], F32)
        with ExitStack() as es:
            sb = lambda n, s, d: self.sb(es, n, s, d)
            gbc = sb("lng", [128, D], F32)
            bbc = sb("lnb", [128, D], F32)
            identb = sb("identb", [128, 128], BF16)
            self.load(gbc, g, g.ap.partition_broadcast(128))
            self.load(bbc, b, b.ap.partition_broadcast(128))
            self.load(identb, self.c_identb, self.c_identb.ap)
            yt = [sb("yt%d" % i, [128, D], F32) for i in range(2)]
            xo = [sb("xo%d" % i, [128, D], F32) for i in range(2)]
            if router is not None:
                wr = sb("wr", [128, KC, NE], F32)
                whi = sb("whi", [128, KC, NE], BF16)
                wlo = sb("wlo", [128, KC, NE], BF16)
                brbc = sb("brbc", [128, NE], F32)
                self.load(wr, w_r, w_r.ap.rearrange("(c p) e -> p c e", p=128))
                self.load(brbc, b_r, b_r.ap.partition_broadcast(128))
                S.op("dve", lambda e: e.tensor_copy(whi.ap[:], wr.ap[:]), reads=[wr], writes=[whi])
                S.op("dve", lambda e: e.tensor_tensor(wlo.ap[:], wr.ap[:], whi.ap[:], ALU.subtract), reads=[wr, whi], writes=[wlo])
                bdh = sb("bdh", [NE, D], BF16)
                bdl = sb("bdl", [NE, D], BF16)
                bdf = yt[1]
                S.dma(lambda e: e.dma_start(out=bdf.ap[0:NE, :], in_=b_dn.ap), reads=[b_dn], writes=[bdf])
                S.op("dve", lambda e: e.tensor_copy(bdh.ap[:], bdf.ap[0:NE, :]), reads=[bdf], writes=[bdh])
                S.op("dve", lambda e: e.tensor_tensor(bdl.ap[:], bdf.ap[0:NE, :], bdh.ap[:], ALU.subtract), reads=[bdf, bdh], writes=[bdl])
                pB = [self.ps(es, "pB%d" % i, [128, 512]) for i in range(2)]
                pL = self.ps(es, "pL", [128, NE])
                pG = self.ps(es, "pG", [NE, 256], BF16)
                xTl = sb("xTl", [128, KC, 128], BF16)
                olo = sb("olo", [128, D], BF16)
                lg = sb("lg", [128, NE], F32)
                ex = sb("ex", [128, NE], F32)
                mk = sb("mk", [128, NE], F32)
                top8 = sb("top8", [128, 8], F32)
                nm1 = sb("nm1", [128, 1], F32)
                den = sb("den", [128, 1], F32)
                gts = sb("gts", [128, NE], F32)
                ghl = sb("ghl", [128, 2, NE], BF16)
                gTh = sb("gTh", [NE, 256], BF16)
            stats = sb("stats", [128, 8, 6], F32)
            mv = sb("mv", [128, 2], F32)
            rs = sb("rs", [128, 1], F32)
            ohi = sb("ohi", [128, D], BF16)
            xTb = sb("xTb", [128, KC, 128], BF16)
            pT = [self.ps(es, "pT%d" % i, [128, 512], BF16) for i in range(2)]
            ncp = 0
            for tt in range(NT // 128):
                a = yt[tt % 2]
                o = xo[tt % 2]
                r0 = tt * 128
                self.load(a, y, y.ap[r0:r0 + 128, :])
                for c in range(8):
                    S.op("dve", lambda e, a=a, c=c: e.bn_stats(stats.ap[:, c, :], a.ap[:, c * 512:(c + 1) * 512]), reads=[a], writes=[stats])
                S.op("dve", lambda e: e.bn_aggr(mv.ap[:], stats.ap[:].rearrange("p a b -> p (a b)")), reads=[stats], writes=[mv])
                S.op("dve", lambda e: e.tensor_scalar(rs.ap[:], mv.ap[:, 1:2], LN_EPS, None, ALU.add), reads=[mv], writes=[rs])
                S.op("act", lambda e: e.activation(rs.ap[:], rs.ap[:], AF.Sqrt), reads=[rs], writes=[rs])
                S.op("dve", lambda e: e.reciprocal(rs.ap[:], rs.ap[:]), reads=[rs], writes=[rs])
                S.op("dve", lambda e, a=a: e.tensor_scalar(a.ap[:], a.ap[:], mv.ap[:, 0:1], rs.ap[:], ALU.subtract, ALU.mult),
                     reads=[a, mv, rs], writes=[a])
                S.op("dve", lambda e, a=a: e.tensor_tensor(a.ap[:], a.ap[:], gbc.ap[:], ALU.mult), reads=[a, gbc], writes=[a])
                S.op("pool", lambda e, a=a, o=o: e.tensor_tensor(o.ap[:], a.ap[:], bbc.ap[:], ALU.add), reads=[a, bbc], writes=[o])
                S.dma(lambda e, o=o, r0=r0: e.dma_start(out=x_out.ap[r0:r0 + 128, :], in_=o.ap[:]), reads=[o], writes=[x_out])
                if xT_out is None and xT_pm is None:
                    continue
                S.op("act", lambda e, o=o: e.copy(ohi.ap[:], o.ap[:]), reads=[o], writes=[ohi])
                srcs = [(ohi, xTb)]
                if router is not None:
                    S.op("dve", lambda e, o=o: e.tensor_tensor(olo.ap[:], o.ap[:], ohi.ap[:], ALU.subtract), reads=[o, ohi], writes=[olo])
                    srcs.append((olo, xTl))
                for (src_, dst_) in srcs:
                    for g4 in range(8):
                        p = pT[ncp % 2]
                        for j in range(4):
                            fc = g4 * 4 + j
                            S.op("pe", lambda e, p=p, src_=src_, fc=fc, j=j: e.transpose(
                                p.ap[:, j * 128:(j + 1) * 128], src_.ap[:, fc * 128:(fc + 1) * 128], identb.ap[:]), reads=[src_, identb], writes=[p])
                        eng = "act" if ncp % 2 == 0 else "dve"
                        ncp += 1
                        if eng == "act":
                            S.op("act", lambda e, p=p, g4=g4, dst_=dst_: e.copy(dst_.ap[:, g4 * 4:(g4 + 1) * 4, :], p.ap[:].rearrange("p (a t) -> p a t", a=4)),
                                 reads=[p], writes=[dst_])
                        else:
                            S.op("dve", lambda e, p=p, g4=g4, dst_=dst_: e.tensor_copy(dst_.ap[:, g4 * 4:(g4 + 1) * 4, :], p.ap[:].rearrange("p (a t) -> p a t", a=4)),
                                 reads=[p], writes=[dst_])
                if xT_pm is not None:
                    for k4 in range(4):
                        S.dma(lambda e, r0=r0, k4=k4: e.dma_start(out=xT_pm[k4].ap.rearrange("p (c t) -> p c t", c=8)[:, :, r0:r0 + 128],
                                                                 in_=xTb.ap[:, 8 * k4:8 * k4 + 8, :]), reads=[xTb], writes=[xT_pm[k4]])
                else:
                    S.dma(lambda e, r0=r0: e.dma_start(out=xT_out.ap.rearrange("(c p) t -> p c t", p=128)[:, :, r0:r0 + 128], in_=xTb.ap[:]),
                          reads=[xTb], writes=[xT_out])
                if router is None:
                    continue
                trip = [(xTb, whi), (xTl, whi), (xTb, wlo)]
                k = 0
                for (xa, wa) in trip:
                    for kc in range(KC):
                        S.op("pe", lambda e, xa=xa, wa=wa, kc=kc, k=k: e.matmul(pL.ap[:], xa.ap[:, kc, :], wa.ap[:, kc, :], start=(k == 0), stop=(k == 3 * KC - 1)),
                             reads=[xa, wa], writes=[pL])
                        k += 1
                S.op("dve", lambda e: e.tensor_tensor(lg.ap[:], pL.ap[:], brbc.ap[:], ALU.add), reads=[pL, brbc], writes=[lg])
                S.op("dve", lambda e: e.max(top8.ap[:], lg.ap[:]), reads=[lg], writes=[top8])
                S.op("dve", lambda e: e.tensor_scalar(mk.ap[:], lg.ap[:], top8.ap[:, 3:4], None, ALU.is_ge), reads=[lg, top8], writes=[mk])
                S.op("dve", lambda e: e.tensor_scalar(nm1.ap[:], top8.ap[:, 0:1], -1.0, None, ALU.mult), reads=[top8], writes=[nm1])
                S.op("act", lambda e: e.activation(ex.ap[:], lg.ap[:], AF.Exp, bias=nm1.ap[:]), reads=[lg, nm1], writes=[ex])
                S.op("dve", lambda e: e.tensor_tensor(ex.ap[:], ex.ap[:], mk.ap[:], ALU.mult), reads=[ex, mk], writes=[ex])
                S.op("dve", lambda e: e.reduce_sum(den.ap[:], ex.ap[:], mybir.AxisListType.X), reads=[ex], writes=[den])
                S.op("dve", lambda e: e.reciprocal(den.ap[:], den.ap[:]), reads=[den], writes=[den])
                S.op("dve", lambda e: e.tensor_scalar(gts.ap[:], ex.ap[:], den.ap[:], None, ALU.mult), reads=[ex, den], writes=[gts])
                S.dma(lambda e, r0=r0: e.dma_start(out=self.gates.ap[r0:r0 + 128, :], in_=gts.ap[:]), reads=[gts], writes=[self.gates])
                S.op("dve", lambda e: e.tensor_copy(ghl.ap[:, 0, :], gts.ap[:]), reads=[gts], writes=[ghl])
                S.op("dve", lambda e: e.tensor_tensor(ghl.ap[:, 1, :], gts.ap[:], ghl.ap[:, 0, :], ALU.subtract), reads=[gts, ghl], writes=[ghl])
                for q_ in range(2):
                    S.op("pe", lambda e, q_=q_: e.transpose(pG.ap[:, q_ * 128:(q_ + 1) * 128], ghl.ap[:, q_, :], identb.ap[:]), reads=[ghl, identb], writes=[pG])
                S.op("act", lambda e: e.copy(gTh.ap[:], pG.ap[:]), reads=[pG], writes=[gTh])
                for dc in range(8):
                    p = pB[dc % 2]
                    cs = slice(dc * 512, (dc + 1) * 512)
                    S.op("pe", lambda e, p=p, cs=cs: e.matmul(p.ap[:], gTh.ap[:, 0:128], bdh.ap[:, cs], start=True, stop=False), reads=[gTh, bdh], writes=[p])
                    S.op("pe", lambda e, p=p, cs=cs: e.matmul(p.ap[:], gTh.ap[:, 128:256], bdh.ap[:, cs], start=False, stop=False), reads=[gTh, bdh], writes=[p])
                    S.op("pe", lambda e, p=p, cs=cs: e.matmul(p.ap[:], gTh.ap[:, 0:128], bdl.ap[:, cs], start=False, stop=True), reads=[gTh, bdl], writes=[p])
                    S.op("act", lambda e, p=p, cs=cs, a=a: e.copy(a.ap[:, cs], p.ap[:]), reads=[p], writes=[a])
                S.dma(lambda e, r0=r0, a=a: e.dma_start(out=self.accinit.ap[r0:r0 + 128, :], in_=a.ap[:]), reads=[a], writes=[self.accinit])
            S.flush()

    def moe(self, x1T, x1, w_gu, b_guT, w_dn, tag):
        actT_all = self.moe_gateup(x1T, w_gu, b_guT, tag)
        return self.moe_down(actT_all, x1, w_dn, tag)

    def moe_gateup(self, x1T, w_gu, b_guT, tag):
        S = self.S
        actT_all = self.dscr("actT_" + tag, [NE, FF, NT], BF16)
        with ExitStack() as es:
            sb = lambda n, s, d: self.sb(es, n, s, d)
            xT = sb("mxT", [128, KC, NT], BF16)
            bgu = sb("bgu", [128, NE, 12], F32)
            wg = [sb("wg%d" % i, [128, KC, 256], BF16) for i in range(3)]
            gluS = sb("gluS", [128, 6, NT], F32)
            actS = [sb("actS%d" % i, [128, 6, NT], BF16) for i in range(2)]
            g1 = [sb("g1_%d" % i, [128, 512], F32) for i in range(2)]
            sg = [sb("sg_%d" % i, [128, 512], F32) for i in range(2)]
            l1 = [sb("l1_%d" % i, [128, 512], F32) for i in range(2)]
            pH = [self.ps(es, "pH%d" % i, [128, 512]) for i in range(8)]
            self.load(bgu, b_guT, b_guT.ap)
            S.op("dve", lambda e: e.tensor_scalar(bgu.ap[:, :, 6:12], bgu.ap[:, :, 6:12], 1.0, None, ALU.add), reads=[bgu], writes=[bgu])
            src = x1T.ap.rearrange("(c p) t -> p c t", p=128)
            S.dma(lambda e: [e.dma_start(out=xT.ap[:, i * 8:(i + 1) * 8, :], in_=src[:, i * 8:(i + 1) * 8, :]) for i in range(4)],
                  reads=[x1T], writes=[xT], n=4)
            nw = 0
            nch = 0
            nev = 0
            for ex_ in range(NE):
                as_ = actS[ex_ % 2]
                for sl in range(6):
                    wb = wg[nw % 3]; nw += 1
                    wsrc = w_gu.ap[ex_, sl]
                    S.dma(lambda e, wb=wb, wsrc=wsrc: [e.dma_start(out=wb.ap[:, 0:16, :], in_=wsrc[:, 0:16, :]),
                                                       e.dma_start(out=wb.ap[:, 16:32, :], in_=wsrc[:, 16:32, :])],
                          reads=[w_gu], writes=[wb], q="pool", n=2)
                    for f2 in range(2):
                        ch = sl * 2 + f2
                        pp = [pH[(2 * nch) % 8], pH[(2 * nch + 1) % 8]]
                        nch += 1
                        for kc in range(KC):
                            for tg in range(2):
                                S.op("pe", lambda e, p=pp[tg], wb=wb, kc=kc, f2=f2, tg=tg: e.matmul(
                                    p.ap[:], wb.ap[:, kc, f2 * 128:(f2 + 1) * 128], xT.ap[:, kc, tg * 512:(tg + 1) * 512],
                                    start=(kc == 0), stop=(kc == KC - 1)), reads=[wb, xT], writes=[pp[tg]])
                        bias = bgu.ap[:, ex_, ch:ch + 1]
                        for tg in range(2):
                            p = pp[tg]
                            ts = slice(tg * 512, (tg + 1) * 512)
                            if ch < 6:
                                a = g1[nev % 2]
                                s_ = sg[nev % 2]
                                nev += 1
                                S.op("dve", lambda e, p=p, a=a, bias=bias: e.tensor_scalar(a.ap[:], p.ap[:], bias, 7.0, ALU.add, ALU.min),
                                     reads=[p, bgu], writes=[a])
                                S.op("act", lambda e, a=a, s_=s_: e.activation(s_.ap[:], a.ap[:], AF.Sigmoid, scale=1.702), reads=[a], writes=[s_])
                                S.op("dve", lambda e, a=a, s_=s_, ch=ch, ts=ts: e.tensor_tensor(gluS.ap[:, ch, ts], a.ap[:], s_.ap[:], ALU.mult),
                                     reads=[a, s_], writes=[gluS])
                            else:
                                j = ch - 6
                                a = l1[nev % 2]
                                nev += 1
                                S.op("dve", lambda e, p=p, a=a, bias=bias: e.tensor_scalar(a.ap[:], p.ap[:], bias, -6.0, ALU.add, ALU.max),
                                     reads=[p, bgu], writes=[a])
                                S.op("dve", lambda e, a=a, j=j, ts=ts, as_=as_: e.scalar_tensor_tensor(
                                    as_.ap[:, j, ts], a.ap[:], 8.0, gluS.ap[:, j, ts], ALU.min, ALU.mult), reads=[a, gluS], writes=[as_])
                S.dma(lambda e, as_=as_, ex_=ex_: e.dma_start(out=actT_all.ap[ex_].rearrange("(j p) t -> p j t", p=128), in_=as_.ap[:]),
                      reads=[as_], writes=[actT_all])
            S.flush()
        return actT_all

    def moe_down(self, actT_all, x1, w_dn, tag):
        S = self.S
        y2 = self.dscr("y2_" + tag, [NT, D], F32)
        DH = D // 2
        with ExitStack() as es:
            sb = lambda n, s, d: self.sb(es, n, s, d)
            acc = sb("acc", [128, NT // 128, DH], F32)
            gts = sb("mg", [128, NT // 128, NE], F32)
            at = [sb("at%d" % i, [128, 6, NT], BF16) for i in range(2)]
            wd = [sb("wd%d" % i, [128, 6, DH], BF16) for i in range(2)]
            xr = [sb("xr%d" % i, [128, 512], F32) for i in range(3)]
            pY = [self.ps(es, "pY%d" % i, [128, 512]) for i in range(8)]
            self.load(gts, self.gates, self.gates.ap.rearrange("(a p) e -> p a e", p=128))
            ny = 0
            nb = 0
            nx = 0
            for dh in range(2):
                d0 = dh * DH
                self.load(acc, self.accinit, self.accinit.ap[:, d0:d0 + DH].rearrange("(a p) d -> p a d", p=128))
                for ex_ in range(NE):
                    ab = at[nb % 2]
                    wb = wd[nb % 2]
                    nb += 1
                    S.dma(lambda e, ab=ab, ex_=ex_: e.dma_start(out=ab.ap[:], in_=actT_all.ap[ex_].rearrange("(j p) t -> p j t", p=128)),
                          reads=[actT_all], writes=[ab])
                    wsrc = w_dn.ap[ex_, :, d0:d0 + DH].rearrange("(c p) n -> p c n", p=128)
                    S.dma(lambda e, wb=wb, wsrc=wsrc: [e.dma_start(out=wb.ap[:, 0:3, :], in_=wsrc[:, 0:3, :]),
                                                       e.dma_start(out=wb.ap[:, 3:6, :], in_=wsrc[:, 3:6, :])],
                          reads=[w_dn], writes=[wb], q="pool", n=2)
                    for tt in range(NT // 128):
                        for dc in range(DH // 512):
                            p = pY[ny % 8]; ny += 1
                            for j in range(6):
                                S.op("pe", lambda e, p=p, ab=ab, wb=wb, j=j, tt=tt, dc=dc: e.matmul(
                                    p.ap[:], ab.ap[:, j, tt * 128:(tt + 1) * 128], wb.ap[:, j, dc * 512:(dc + 1) * 512],
                                    start=(j == 0), stop=(j == 5)), reads=[ab, wb], writes=[p])
                            cs = slice(dc * 512, (dc + 1) * 512)
                            S.op("dve", lambda e, p=p, tt=tt, cs=cs, ex_=ex_: e.scalar_tensor_tensor(
                                acc.ap[:, tt, cs], p.ap[:], gts.ap[:, tt, ex_:ex_ + 1], acc.ap[:, tt, cs], ALU.mult, ALU.add),
                                reads=[p, gts, acc], writes=[acc])
                for tt in range(NT // 128):
                    for dc in range(DH // 512):
                        xb = xr[nx % 3]; nx += 1
                        r0 = tt * 128
                        cs = slice(dc * 512, (dc + 1) * 512)
                        gs = slice(d0 + dc * 512, d0 + (dc + 1) * 512)
                        S.dma(lambda e, xb=xb, r0=r0, gs=gs: e.dma_start(out=xb.ap[:], in_=x1.ap[r0:r0 + 128, gs]), reads=[x1], writes=[xb])
                        S.op("dve", lambda e, xb=xb, tt=tt, cs=cs: e.scalar_tensor_tensor(
                            xb.ap[:], xb.ap[:], ALPHA, acc.ap[:, tt, cs], ALU.mult, ALU.add), reads=[xb, acc], writes=[xb])
                        S.dma(lambda e, xb=xb, r0=r0, gs=gs: e.dma_start(out=y2.ap[r0:r0 + 128, gs], in_=xb.ap[:]), reads=[xb], writes=[y2])
            S.flush()
        return y2

    def moe_old(self, x1T, x1, w_gu, b_guT, w_dn, tag):
        S = self.S
        y2 = self.dscr("y2_" + tag, [NT, D], F32)
        TP = 512
        with ExitStack() as es:
            sb = lambda n, s, d: self.sb(es, n, s, d)
            xT = sb("mxT", [128, KC, TP], BF16)
            acc = sb("acc", [128, TP // 128, D], F32)
            gts = sb("mg", [128, TP // 128, NE], F32)
            bgu = sb("bgu", [128, NE, 12], F32)
            wg = [sb("wg%d" % i, [128, KC, 256], BF16) for i in range(2)]
            wd = [sb("wd%d" % i, [128, 6, 1024], BF16) for i in range(2)]
            gluS = sb("gluS", [128, 6, TP], F32)
            actT = sb("actT", [128, 6, TP], BF16)
            g1 = [sb("g1_%d" % i, [128, TP], F32) for i in range(2)]
            sg = [sb("sg_%d" % i, [128, TP], F32) for i in range(2)]
            l1 = [sb("l1_%d" % i, [128, TP], F32) for i in range(2)]
            xr = [sb("xr%d" % i, [128, 512], F32) for i in range(2)]
            pH = [self.ps(es, "pH%d" % i, [128, 512]) for i in range(4)]
            pY = [self.ps(es, "pY%d" % i, [128, 512]) for i in range(4)]
            self.load(bgu, b_guT, b_guT.ap)
            S.op("dve", lambda e: e.tensor_scalar(bgu.ap[:, :, 6:12], bgu.ap[:, :, 6:12], 1.0, None, ALU.add), reads=[bgu], writes=[bgu])
            nw = 0
            nd = 0
            nh = 0
            ny = 0
            for ps_ in range(NT // TP):
                t0 = ps_ * TP
                src = x1T.ap[:, t0:t0 + TP].rearrange("(c p) t -> p c t", p=128)
                S.dma(lambda e, src=src: [e.dma_start(out=xT.ap[:, i * 8:(i + 1) * 8, :], in_=src[:, i * 8:(i + 1) * 8, :]) for i in range(4)],
                      reads=[x1T], writes=[xT], n=4)
                self.load(gts, self.gates, self.gates.ap[t0:t0 + TP, :].rearrange("(a p) e -> p a e", p=128))
                self.load(acc, self.accinit, self.accinit.ap[t0:t0 + TP, :].rearrange("(a p) d -> p a d", p=128))
                for ex_ in range(NE):
                    for sl in range(6):
                        wb = wg[nw % 2]; nw += 1
                        wsrc = w_gu.ap[ex_, :, sl * 256:(sl + 1) * 256].rearrange("(c p) n -> p c n", p=128)
                        S.dma(lambda e, wb=wb, wsrc=wsrc: [e.dma_start(out=wb.ap[:, 0:16, :], in_=wsrc[:, 0:16, :]),
                                                           e.dma_start(out=wb.ap[:, 16:32, :], in_=wsrc[:, 16:32, :])],
                              reads=[w_gu], writes=[wb], q="pool", n=2)
                        for f2 in range(2):
                            ch = sl * 2 + f2
                            p = pH[nh % 4]; nh += 1
                            for kc in range(KC):
                                S.op("pe", lambda e, p=p, wb=wb, kc=kc, f2=f2: e.matmul(p.ap[:], wb.ap[:, kc, f2 * 128:(f2 + 1) * 128], xT.ap[:, kc, :],
                                                                                     start=(kc == 0), stop=(kc == KC - 1)), reads=[wb, xT], writes=[p])
                            bias = bgu.ap[:, ex_, ch:ch + 1]
                            if ch < 6:
                                a = g1[ch % 2]
                                s_ = sg[ch % 2]
                                S.op("dve", lambda e, p=p, a=a, bias=bias: e.tensor_scalar(a.ap[:], p.ap[:], bias, 7.0, ALU.add, ALU.min),
                                     reads=[p, bgu], writes=[a])
                                S.op("act", lambda e, a=a, s_=s_: e.activation(s_.ap[:], a.ap[:], AF.Sigmoid, scale=1.702), reads=[a], writes=[s_])
                                S.op("pool", lambda e, a=a, s_=s_, ch=ch: e.tensor_tensor(gluS.ap[:, ch, :], a.ap[:], s_.ap[:], ALU.mult),
                                     reads=[a, s_], writes=[gluS])
                            else:
                                j = ch - 6
                                a = l1[ch % 2]
                                S.op("dve", lambda e, p=p, a=a, bias=bias: e.tensor_scalar(a.ap[:], p.ap[:], bias, -6.0, ALU.add, ALU.max),
                                     reads=[p, bgu], writes=[a])
                                S.op("dve", lambda e, a=a, j=j: e.scalar_tensor_tensor(actT.ap[:, j, :], a.ap[:], 8.0, gluS.ap[:, j, :], ALU.min, ALU.mult),
                                     reads=[a, gluS], writes=[actT])
                    for dq in range(4):
                        wb = wd[nd % 2]; nd += 1
                        wsrc = w_dn.ap[ex_, :, dq * 1024:(dq + 1) * 1024].rearrange("(c p) n -> p c n", p=128)
                        S.dma(lambda e, wb=wb, wsrc=wsrc: [e.dma_start(out=wb.ap[:, 0:3, :], in_=wsrc[:, 0:3, :]),
                                                           e.dma_start(out=wb.ap[:, 3:6, :], in_=wsrc[:, 3:6, :])],
                              reads=[w_dn], writes=[wb], q="pool", n=2)
                        for tt in range(TP // 128):
                            for dc in range(2):
                                p = pY[ny % 4]; ny += 1
                                for j in range(6):
                                    S.op("pe", lambda e, p=p, wb=wb, j=j, tt=tt, dc=dc: e.matmul(
                                        p.ap[:], actT.ap[:, j, tt * 128:(tt + 1) * 128], wb.ap[:, j, dc * 512:(dc + 1) * 512],
                                        start=(j == 0), stop=(j == 5)), reads=[actT, wb], writes=[p])
                                cs = slice(dq * 1024 + dc * 512, dq * 1024 + (dc + 1) * 512)
                                S.op("dve", lambda e, p=p, tt=tt, cs=cs, ex_=ex_: e.scalar_tensor_tensor(
                                    acc.ap[:, tt, cs], p.ap[:], gts.ap[:, tt, ex_:ex_ + 1], acc.ap[:, tt, cs], ALU.mult, ALU.add),
                                    reads=[p, gts, acc], writes=[acc])
                for tt in range(TP // 128):
                    for dc in range(8):
                        xb = xr[(tt * 8 + dc) % 2]
                        r0 = t0 + tt * 128
                        cs = slice(dc * 512, (dc + 1) * 512)
                        S.dma(lambda e, xb=xb, r0=r0, cs=cs: e.dma_start(out=xb.ap[:], in_=x1.ap[r0:r0 + 128, cs]), reads=[x1], writes=[xb])
                        S.op("dve", lambda e, xb=xb, tt=tt, cs=cs: e.scalar_tensor_tensor(
                            xb.ap[:], xb.ap[:], ALPHA, acc.ap[:, tt, cs], ALU.mult, ALU.add), reads=[xb, acc], writes=[xb])
                        S.dma(lambda e, xb=xb, r0=r0, cs=cs: e.dma_start(out=y2.ap[r0:r0 + 128, cs], in_=xb.ap[:]), reads=[xb], writes=[y2])
            S.flush()
        return y2

    def sb_proj(self, xT_prev, xT_own, w_kv, w_q, pm=False):
        S = self.S
        self.kT = self.dscr("kT", [D, 2 * NT], BF16)
        self.vtm = self.dscr("vtm", [2 * NT, D], BF16)
        self.sqT = self.dscr("sqT", [D, NT], BF16)
        with ExitStack() as es:
            xT = self.sb(es, "xT", [128, KC, NT], BF16)
            wsl = [self.sb(es, "wsl%d" % i, [128, KC, 512], BF16) for i in range(2)]
            pss = [self.ps(es, "pp%d" % i, [128, 512]) for i in range(4)]
            stb = [self.sb(es, "stb%d" % i, [128, 512], BF16) for i in range(3)]
            self.lin_state = {"w": 0, "p": 0}
            cnt = {"b": 0}
            for (srcb, tok0, own) in ((xT_own, NT, True), (xT_prev, 0, False)):
                if pm:
                    for k4 in range(4):
                        S.dma(lambda e, k4=k4, srcb=srcb: e.dma_start(out=xT.ap[:, 8 * k4:8 * k4 + 8, :],
                                                                     in_=srcb[k4].ap[0:128, :].rearrange("p (c t) -> p c t", c=8)),
                              reads=[srcb[k4]], writes=[xT])
                else:
                    src = srcb.ap.rearrange("(c p) t -> p c t", p=128)
                    S.dma(lambda e, src=src: [e.dma_start(out=xT.ap[:, i * 8:(i + 1) * 8, :], in_=src[:, i * 8:(i + 1) * 8, :]) for i in range(4)],
                          reads=[srcb], writes=[xT], n=4)

                def ep_k(ps, f0, nf, tg, tok0=tok0):
                    sbuf = stb[cnt["b"] % 3]; cnt["b"] += 1
                    S.op("act", lambda e: e.copy(sbuf.ap[:], ps.ap[:]), reads=[ps], writes=[sbuf])
                    S.dma(lambda e: e.dma_start(out=self.kT.ap[f0:f0 + 128, tok0 + tg * 512:tok0 + (tg + 1) * 512], in_=sbuf.ap[:]),
                          reads=[sbuf], writes=[self.kT])

                def ep_v(ps, tt, c0, ncw, tok0=tok0):
                    sbuf = stb[cnt["b"] % 3]; cnt["b"] += 1
                    S.op("dve", lambda e: e.tensor_copy(sbuf.ap[:], ps.ap[:]), reads=[ps], writes=[sbuf])
                    r0 = tok0 + tt * 128
                    S.dma(lambda e: e.dma_start(out=self.vtm.ap[r0:r0 + 128, c0 - D:c0 - D + 512], in_=sbuf.ap[:]),
                          reads=[sbuf], writes=[self.vtm])

                def ep_q(ps, f0, nf, tg):
                    sbuf = stb[cnt["b"] % 3]; cnt["b"] += 1
                    S.op("act", lambda e: e.activation(sbuf.ap[:], ps.ap[:], AF.Copy, scale=SB_DH ** -0.5), reads=[ps], writes=[sbuf])
                    S.dma(lambda e: e.dma_start(out=self.sqT.ap[f0:f0 + 128, tg * 512:(tg + 1) * 512], in_=sbuf.ap[:]),
                          reads=[sbuf], writes=[self.sqT])
                self.linear(es, xT, 0, NT, w_kv, w_kv.ap, [(c, 512) for c in range(0, D, 512)], "fm", ep_k, wsl, pss)
                self.linear(es, xT, 0, NT, w_kv, w_kv.ap, [(c, 512) for c in range(D, 2 * D, 512)], "tm", ep_v, wsl, pss)
                if own:
                    self.linear(es, xT, 0, NT, w_q, w_q.ap, [(c, 512) for c in range(0, D, 512)], "fm", ep_q, wsl, pss)
            S.flush()

    def sb_attn(self):
        S = self.S
        self.oT = self.dscr("oT", [D, NT], BF16)
        with ExitStack() as es:
            sb = lambda n, s, d: self.sb(es, n, s, d)
            L = sb("L", [128, 128], F32)
            ones = sb("ones", [128, 128], F32)
            mask = sb("mask", [128, 4, 512], F32)
            hpb = sb("hpb", [128, 1], F32)
            self.load(L, self.c_L, self.c_L.ap)
            self.load(ones, self.c_ones, self.c_ones.ap)
            self.load(mask, self.c_mask, self.c_mask.ap)
            self.load(hpb, self.c_hpbias, self.c_hpbias.ap)
            kTh = [sb("kTh%d" % i, [128, 2 * NT], BF16) for i in range(2)]
            vh = [sb("vh%d" % i, [128, 16, 128], BF16) for i in range(2)]
            qTh = [sb("qTh%d" % i, [128, NT], BF16) for i in range(2)]
            ee = [sb("ee%d" % i, [128, 512], F32) for i in range(2)]
            spb = [sb("spb%d" % i, [128, 512], F32) for i in range(3)]
            accSs = [sb("accS%d" % i, [128, 512], F32) for i in range(2)]
            d1 = [sb("d1_%d" % i, [128, 512], F32) for i in range(4)]
            wf = [sb("wf%d" % i, [128, 512], F32) for i in range(2)]
            wT = [sb("wT%d" % i, [128, 512], BF16) for i in range(3)]
            osb = [sb("osb%d" % i, [128, 512], BF16) for i in range(2)]
            pZ = [self.ps(es, "pZ%d" % i, [128, 512]) for i in range(2)]
            pX = [self.ps(es, "pX%d" % i, [128, 512]) for i in range(2)]
            pXb = [self.ps(es, "pXb%d" % i, [128, 512]) for i in range(2)]
            pO = [self.ps(es, "pO%d" % i, [128, 512]) for i in range(2)]

            def head_loads(h):
                hb = h % 2
                self.load(kTh[hb], self.kT, self.kT.ap[h * 128:(h + 1) * 128, :])
                self.load(vh[hb], self.vtm, self.vtm.ap[:, h * 128:(h + 1) * 128].rearrange("(a p) d -> p a d", p=128))
                self.load(qTh[hb], self.sqT, self.sqT.ap[h * 128:(h + 1) * 128, :])

            tiles = []
            g = 0
            for h in range(SB_H):
                for qg in range(2):
                    blocks = [("own", kb) for kb in range(4 * qg + 3, -1, -1)] + [("prev", kb) for kb in range(7, -1, -1)]
                    for bi_, (kind, kb) in enumerate(blocks):
                        mj = kb - 4 * qg if (kind == "own" and kb >= 4 * qg) else None
                        tiles.append(dict(i=len(tiles), h=h, hb=h % 2, qg=qg, kind=kind, kidx=kb + (8 if kind == "own" else 0), mj=mj,
                                          first=(bi_ == 0), last=(bi_ == len(blocks) - 1), g=g,
                                          head_last=(qg == 1 and bi_ == len(blocks) - 1)))
                    g += 1

            def S1(t):
                i, hb = t["i"], t["hb"]
                pz, e_, s_, d_ = pZ[i % 2], ee[i % 2], spb[i % 3], d1[i % 4]
                kidx, qg = t["kidx"], t["qg"]
                S.op("pe", lambda e: e.matmul(pz.ap[:], kTh[hb].ap[:, kidx * 128:(kidx + 1) * 128], qTh[hb].ap[:, qg * 512:(qg + 1) * 512],
                                              start=True, stop=True), reads=[kTh[hb], qTh[hb]], writes=[pz])
                S.op("act", lambda e: e.activation(e_.ap[:], pz.ap[:], AF.Exp), reads=[pz], writes=[e_])
                S.op("act", lambda e: e.activation(s_.ap[:], e_.ap[:], AF.Ln, bias=1.0), reads=[e_], writes=[s_])

            def S1b(t):
                i = t["i"]
                pz, s_, d_ = pZ[i % 2], spb[i % 3], d1[i % 4]
                S.op("dve", lambda e: e.tensor_tensor(d_.ap[:], pz.ap[:], s_.ap[:], ALU.subtract), reads=[pz, s_], writes=[d_])

            def S2(t):
                i = t["i"]
                s_, px, py = spb[i % 3], pX[i % 2], pXb[i % 2]
                mj = t["mj"]
                if mj is not None:
                    S.op("dve", lambda e: e.tensor_tensor(s_.ap[:], s_.ap[:], mask.ap[:, mj, :], ALU.mult), reads=[s_, mask], writes=[s_])
                S.op("pe", lambda e: e.matmul(px.ap[:], L.ap[:], s_.ap[:], start=True, stop=True), reads=[L, s_], writes=[px])
                accP, accN = accSs[(i + 1) % 2], accSs[i % 2]
                if not t["first"]:
                    S.op("pe", lambda e: e.matmul(py.ap[:], ones.ap[:], accP.ap[:], start=True, stop=True), reads=[ones, accP], writes=[py])
                if not t["last"]:
                    if t["first"]:
                        S.op("pool", lambda e: e.tensor_copy(accN.ap[:], s_.ap[:]), reads=[s_], writes=[accN])
                    else:
                        S.op("dve", lambda e: e.tensor_tensor(accN.ap[:], accP.ap[:], s_.ap[:], ALU.add), reads=[s_, accP], writes=[accN])

            def S3(t):
                i, hb, h, qg = t["i"], t["hb"], t["h"], t["qg"]
                d_, px, py, w_, wt = d1[i % 4], pX[i % 2], pXb[i % 2], wf[i % 2], wT[i % 3]
                po, ob = pO[t["g"] % 2], osb[t["g"] % 2]
                mj, kidx = t["mj"], t["kidx"]
                S.op("dve", lambda e: e.tensor_tensor(d_.ap[:], d_.ap[:], px.ap[:], ALU.subtract), reads=[d_, px], writes=[d_])
                if not t["first"]:
                    S.op("dve", lambda e: e.tensor_tensor(d_.ap[:], d_.ap[:], py.ap[:], ALU.subtract), reads=[d_, py], writes=[d_])
                if mj is not None:
                    S.op("act", lambda e: e.activation(w_.ap[:], d_.ap[:], AF.Exp), reads=[d_], writes=[w_])
                elif t["kind"] == "prev":
                    S.op("act", lambda e: e.activation(wt.ap[:], d_.ap[:], AF.Exp, bias=hpb.ap[:]), reads=[d_, hpb], writes=[wt])
                else:
                    S.op("act", lambda e: e.activation(wt.ap[:], d_.ap[:], AF.Exp), reads=[d_], writes=[wt])

            def S3b(t):
                i, hb, h, qg = t["i"], t["hb"], t["h"], t["qg"]
                wt = wT[i % 3]
                po, ob = pO[t["g"] % 2], osb[t["g"] % 2]
                kidx = t["kidx"]
                first, last = t["first"], t["last"]
                if t["mj"] is not None:
                    w_, mj = wf[i % 2], t["mj"]
                    S.op("dve", lambda e: e.tensor_tensor(wt.ap[:], w_.ap[:], mask.ap[:, mj, :], ALU.mult), reads=[w_, mask], writes=[wt])
                S.op("pe", lambda e: e.matmul(po.ap[:], vh[hb].ap[:, kidx, :], wt.ap[:], start=first, stop=last), reads=[vh[hb], wt], writes=[po])
                if last:
                    S.op("act", lambda e: e.copy(ob.ap[:], po.ap[:]), reads=[po], writes=[ob])
                    S.dma(lambda e: e.dma_start(out=self.oT.ap[h * 128:(h + 1) * 128, qg * 512:(qg + 1) * 512], in_=ob.ap[:]),
                          reads=[ob], writes=[self.oT])
                if t["head_last"] and h + 2 < SB_H:
                    head_loads(h + 2)

            head_loads(0)
            head_loads(1)
            n = len(tiles)
            for step in range(n + 2):
                if step < n:
                    S1(tiles[step])
                if 0 <= step - 2 < n:
                    S3(tiles[step - 2])
                if 0 <= step - 1 < n:
                    S2(tiles[step - 1])
                if step < n:
                    S1b(tiles[step])
                if 0 <= step - 2 < n:
                    S3b(tiles[step - 2])
            S.flush()


def _declare_layer_common(P, l, big=True):
    w = {}
    w["router_w"] = P.din("router_w%d" % l, [D, NE])
    w["router_b"] = P.din("router_b%d" % l, [1, NE])
    if big:
        w["w_gu"] = P.din("w_gu%d" % l, [NE, 6, 128, KC, 256])
        w["w_dn"] = P.din("w_dn%d" % l, [NE, FF, D])
    w["b_guT"] = P.din("b_guT%d" % l, [128, NE, 12])
    w["b_dn"] = P.din("b_dn%d" % l, [NE, D])
    for nm in ("ln1_g", "ln1_b", "ln2_g", "ln2_b"):
        w[nm] = P.din("%s%d" % (nm, l), [1, D])
    return w


def build_layer0(nph=99):
    P = Prog([0])
    P.declare_consts()
    xT_all = P.din("xT_all", [D, 2 * NT])
    x_own = P.din("x_own", [NT, D])
    w_in = P.din("gla_w_in", [D, GIN])
    w_g2 = P.din("gla_w_gate2", [16, QK])
    b_g2 = P.din("gla_b_gate2", [1, QK])
    ng = P.din("gla_norm_g", [1, DV])
    w_out = P.din("gla_w_out", [GV, D])
    if nph >= 4:
        w = _declare_layer_common(P, 0, big=(nph >= 5))
    x_l0 = P.dout("x_l0", [NT, D])
    x_l0T = P.dout("x_l0T", [D, NT], BF16)
    P.gla_inproj(xT_all, w_in)
    if nph >= 2:
        P.gla_scan(w_g2, b_g2, ng)
    if nph >= 3:
        y = P.outproj_residual(P.ogT, w_out, x_own, "a0")
    if nph >= 4:
        x1 = P.dscr("x1_0", [NT, D], F32)
        x1T = P.dscr("x1T_0", [D, NT], BF16)
        import os
        dbg = int(os.environ.get("LNDBG", "2"))
        P.ln_phase(y, w["ln1_g"], w["ln1_b"], x1, x1T if dbg >= 1 else None, (w["router_w"], w["router_b"], w["b_dn"]) if dbg >= 2 else None, tag="0")
    if nph >= 5:
        y2 = P.moe(x1T, x1, w["w_gu"], w["b_guT"], w["w_dn"], "0")
    if nph >= 6:
        P.ln_phase(y2, w["ln2_g"], w["ln2_b"], x_l0, x_l0T, None, tag="0b")
    if nph < 6:
        src = {1: P.k_tm, 2: None, 3: None, 4: None, 5: None}[nph] if nph == 1 else None
        with ExitStack() as es:
            t = P.sb(es, "dbg", [128, D], F32)
            tb = P.sb(es, "dbgb", [128, D], BF16)
            for tt in range(8):
                if nph == 1:
                    P.S.dma(lambda e, tt=tt: e.dma_start(out=t.ap[:, 0:QK], in_=P.k_tm.ap[NT + tt * 128:NT + (tt + 1) * 128, :]), reads=[P.k_tm], writes=[t])
                elif nph == 2:
                    P.S.dma(lambda e, tt=tt: e.dma_start(out=tb.ap[:, 0:NT], in_=P.ogT.ap[tt * 128:(tt + 1) * 128, :]), reads=[P.ogT], writes=[tb])
                    P.S.op("dve", lambda e: e.tensor_copy(t.ap[:, 0:NT], tb.ap[:, 0:NT]), reads=[tb], writes=[t])
                elif nph == 3:
                    P.S.dma(lambda e, tt=tt: e.dma_start(out=t.ap[:], in_=y.ap[tt * 128:(tt + 1) * 128, :]), reads=[y], writes=[t])
                elif nph == 4:
                    P.S.dma(lambda e, tt=tt: e.dma_start(out=t.ap[:], in_=x1.ap[tt * 128:(tt + 1) * 128, :]), reads=[x1], writes=[t])
                elif nph == 5:
                    P.S.dma(lambda e, tt=tt: e.dma_start(out=t.ap[:], in_=y2.ap[tt * 128:(tt + 1) * 128, :]), reads=[y2], writes=[t])
                P.S.dma(lambda e, tt=tt: e.dma_start(out=x_l0.ap[tt * 128:(tt + 1) * 128, :], in_=t.ap[:]), reads=[t], writes=[x_l0])
            P.S.flush()
    P.S.close()
    return P


def build_layer1():
    P = Prog([1])
    P.declare_consts()
    xT_prev = P.din("xT_prev", [D, NT], BF16)
    xT_own = P.din("xT_own", [D, NT], BF16)
    x_own = P.din("x_own", [NT, D])
    w_kv = P.din("shared_w_kv", [D, 2 * D])
    w_q = P.din("sb_w_q", [D, D])
    w_out = P.din("sb_w_out", [D, D])
    w = _declare_layer_common(P, 1)
    out = P.dout("out", [NT, D])
    P.sb_proj(xT_prev, xT_own, w_kv, w_q)
    P.sb_attn()
    y = P.outproj_residual(P.oT, w_out, x_own, "a1")
    x1 = P.dscr("x1_1", [NT, D], F32)
    x1T = P.dscr("x1T_1", [D, NT], BF16)
    P.ln_phase(y, w["ln1_g"], w["ln1_b"], x1, x1T, (w["router_w"], w["router_b"], w["b_dn"]), tag="1")
    y2 = P.moe(x1T, x1, w["w_gu"], w["b_guT"], w["w_dn"], "1")
    P.ln_phase(y2, w["ln2_g"], w["ln2_b"], out, None, None, tag="1b")
    P.S.close()
    return P


def _consts(h):
    i = np.arange(128)
    c = {}
    c["c_ident"] = np.eye(128, dtype=np.float32)
    c["c_identb"] = np.eye(128, dtype=np.float32).astype(ml_dtypes.bfloat16)
    c["c_U"] = ((i[:, None] > i[None, :]) & ((i[:, None] // 64) == (i[None, :] // 64))).astype(np.float32)
    c["c_ind"] = np.stack([(i // 64 == 0), (i // 64 == 1)], axis=1).astype(np.float32)
    c["c_L"] = (i[:, None] > i[None, :]).astype(np.float32)
    c["c_ones"] = np.ones((128, 128), np.float32)
    m = np.zeros((128, 4, 512), np.float32)
    tq = np.arange(512)
    for j in range(4):
        jb = tq // 128
        tri = (i[:, None] < (tq % 128)[None, :])
        m[:, j, :] = np.where(jb[None, :] < j, 0.0, np.where(jb[None, :] == j, tri, 1.0))
    c["c_mask"] = m
    c["c_hpbias"] = np.full((128, 1), 0.0 if h == 1 else -30000.0, np.float32)
    return c


def _layer_common_inputs(l, inp):
    d = {}
    d["router_w%d" % l] = np.ascontiguousarray(inp["router_w"][l])
    d["router_b%d" % l] = np.ascontiguousarray(inp["router_b"][l][None, :])
    d["w_gu%d" % l] = np.ascontiguousarray(inp["moe_w_gate_up"][l].reshape(NE, KC, 128, 6, 256).transpose(0, 3, 2, 1, 4))
    bgu = inp["moe_b_gate_up"][l]
    d["b_guT%d" % l] = np.ascontiguousarray(bgu.reshape(NE, 12, 128).transpose(2, 0, 1))
    d["w_dn%d" % l] = np.ascontiguousarray(inp["moe_w_down"][l])
    d["b_dn%d" % l] = np.ascontiguousarray(inp["moe_b_down"][l])
    for nm in ("ln1_g", "ln1_b", "ln2_g", "ln2_b"):
        d["%s%d" % (nm, l)] = np.ascontiguousarray(inp[nm][l][None, :])
    return d


def build_fused():
    P = Prog([0, 1])
    P.declare_consts()
    xT_all = P.din("xT_all", [D, 2 * NT])
    x_own = P.din("x_own", [NT, D])
    w_in = P.din("gla_w_in", [D, GIN])
    w_g2 = P.din("gla_w_gate2", [16, QK])
    b_g2 = P.din("gla_b_gate2", [1, QK])
    ng = P.din("gla_norm_g", [1, DV])
    w_out0 = P.din("gla_w_out", [GV, D])
    w0 = _declare_layer_common(P, 0)
    w_kv = P.din("shared_w_kv", [D, 2 * D])
    w_q = P.din("sb_w_q", [D, D])
    w_out1 = P.din("sb_w_out", [D, D])
    w1 = _declare_layer_common(P, 1)
    out = P.dout("out", [NT, D])
    S = P.S
    P.gla_inproj(xT_all, w_in)
    P.gla_scan(w_g2, b_g2, ng)
    y = P.outproj_residual(P.ogT, w_out0, x_own, "a0")
    x1 = P.dscr("x1_0", [NT, D], F32)
    x1T = P.dscr("x1T_0", [D, NT], BF16)
    P.ln_phase(y, w0["ln1_g"], w0["ln1_b"], x1, x1T, (w0["router_w"], w0["router_b"], w0["b_dn"]), tag="0")
    y2 = P.moe(x1T, x1, w0["w_gu"], w0["b_guT"], w0["w_dn"], "0")
    x_l0 = P.dscr("x_l0", [NT, D], F32)
    src = [P.dscr("xsrc%d" % k, [128, 8192], BF16) for k in range(4)]
    dst = [P.dscr("xdst%d" % k, [256, 8192], BF16) for k in range(4)]
    P.ln_phase(y2, w0["ln2_g"], w0["ln2_b"], x_l0, None, None, tag="0b", xT_pm=src)
    rg = [[0, 1], [2, 3], [4, 5], [6, 7]]
    for k in range(4):
        S.dma(lambda e, k=k: e.collective_compute("AllGather", ALU.bypass, replica_groups=rg,
                                                  ins=[src[k].ap.opt()], outs=[dst[k].ap.opt()]),
              reads=[src[k]], writes=[dst[k]], q="pool", amt=1)
    S.flush()
    P.sb_proj(dst, src, w_kv, w_q, pm=True)
    P.sb_attn()
    y = P.outproj_residual(P.oT, w_out1, x_l0, "a1")
    x1b = P.dscr("x1_1", [NT, D], F32)
    x1Tb = P.dscr("x1T_1", [D, NT], BF16)
    P.ln_phase(y, w1["ln1_g"], w1["ln1_b"], x1b, x1Tb, (w1["router_w"], w1["router_b"], w1["b_dn"]), tag="1")
    y2 = P.moe(x1Tb, x1b, w1["w_gu"], w1["b_guT"], w1["w_dn"], "1")
    P.ln_phase(y2, w1["ln2_g"], w1["ln2_b"], out, None, None, tag="1b")
    P.S.close()
    return P


_CACHE = {}


def kernel(**inp):
    inp = {k: np.asarray(v) for k, v in inp.items()}
    x = inp["x"]
    B = x.shape[0]
    ncore = 2 * B
    if "pf" not in _CACHE:
        _CACHE["pf"] = build_fused()
    P = _CACHE["pf"]
    com = _layer_common_inputs(0, inp)
    com.update(_layer_common_inputs(1, inp))
    com.update({"gla_w_in": np.ascontiguousarray(inp["gla_w_in"][0]), "gla_w_gate2": np.ascontiguousarray(inp["gla_w_gate2"][0]),
                "gla_b_gate2": np.ascontiguousarray(inp["gla_b_gate2"][0][None, :]), "gla_norm_g": np.ascontiguousarray(inp["gla_norm_g"][0][None, :]),
                "gla_w_out": np.ascontiguousarray(inp["gla_w_out"][0]),
                "shared_w_kv": np.ascontiguousarray(inp["shared_w_kv"]), "sb_w_q": np.ascontiguousarray(inp["sb_w_q"][0]),
                "sb_w_out": np.ascontiguousarray(inp["sb_w_out"][0])})
    maps = []
    for c in range(ncore):
        b, h = c // 2, c % 2
        m = dict(com)
        m.update(_consts(h))
        xb = x[b]
        xT_all = np.zeros((D, 2 * NT), np.float32)
        xT_all[:, NT:] = xb[h * NT:(h + 1) * NT].T
        if h == 1:
            xT_all[:, :NT] = xb[:NT].T
        m["xT_all"] = xT_all
        m["x_own"] = np.ascontiguousarray(xb[h * NT:(h + 1) * NT])
        maps.append(m)
    r = run_bass_kernel_spmd(P.nc, maps, core_ids=list(range(ncore))).results
    out = np.zeros((B, 2 * NT, D), np.float32)
    for c in range(ncore):
        b, h = c // 2, c % 2
        out[b, h * NT:(h + 1) * NT] = r[c]["out"]
    return out
```
